# Optimizing a Trainium2 kernel written in Bass

```python
import jax, jax.numpy as jnp
from jax import lax
import numpy as np

D_MODEL = 1024
BATCH = 32
SEQ = 2048
DEPTH = 1

CTX_LEN = 256
GRID_W = 64

D_MIX = D_MODEL
DA = D_MIX // 2
NA = 64
HA = DA // NA
DECAY_LORA = 64
AAA_LORA = 64
GATE_LORA = 128
RWKV_COLS = 3 * DA + 2 * DECAY_LORA + 2 * AAA_LORA + GATE_LORA
RWKV_EPS = 64e-5

DB = D_MIX - DA
HB = 4
KB = DB // HB
GDN_CONV = 5
GDN_CHUNK = 64
GDN_COLS = 4 * DB + 4 * HB
IN_COLS = RWKV_COLS + GDN_COLS

N_EXPERTS = 256
TOP_K = 8
N_GROUPS = 8
TOPK_GROUPS = 4
D_EXPERT = 256
D_SHARED = 256
ROUTED_SCALE = 2.5
MOE_BLOCK = 256
NORM_EPS = 1e-6

kernel_name = 'hybrid_rwkv7_gdn_moe_dit_layer'


def rmsnorm(x, g):
    xf = x.astype(jnp.float32)
    y = xf * lax.rsqrt(jnp.mean(xf * xf, axis=-1, keepdims=True) + NORM_EPS)
    return (y * g.astype(jnp.float32)).astype(x.dtype)


def l2norm(x):
    xf = x.astype(jnp.float32)
    return xf * lax.rsqrt(jnp.sum(xf * xf, axis=-1, keepdims=True) + 1e-6)


def shift_seq(p):
    h = p.shape[-1] // 2
    prev = jnp.pad(p[:, :-1, :h], ((0, 0), (1, 0), (0, 0)))
    nxt = jnp.pad(p[:, 1:, h:], ((0, 0), (0, 1), (0, 0)))
    return jnp.concatenate([prev, nxt], axis=-1)


def shift_grid(p):
    B, T, C = p.shape
    rows = T // GRID_W
    q = C // 4
    g = p.reshape(B, rows, GRID_W, C)
    left = jnp.pad(g[:, :, :-1, :q], ((0, 0), (0, 0), (1, 0), (0, 0)))
    right = jnp.pad(g[:, :, 1:, q:2 * q], ((0, 0), (0, 0), (0, 1), (0, 0)))
    up = jnp.pad(g[:, :-1, :, 2 * q:3 * q], ((0, 0), (1, 0), (0, 0), (0, 0)))
    down = jnp.pad(g[:, 1:, :, 3 * q:], ((0, 0), (0, 1), (0, 0), (0, 0)))
    return jnp.concatenate([left, right, up, down], axis=-1).reshape(B, T, C)


def rwkv7_prepare(p, shifted, mu, w0, w_up, a0, a_up, g_up, k_k, k_a):
    p = (p + mu * (shifted - p)).astype(jnp.float32)
    p = jnp.swapaxes(p, 0, 1)
    T, B = p.shape[:2]
    cuts = [DA, 2 * DA, 3 * DA, 3 * DA + 2 * DECAY_LORA, 3 * DA + 2 * DECAY_LORA + 2 * AAA_LORA]
    r, k, v, wl, al, gl = jnp.split(p, cuts, axis=-1)
    wl = wl.reshape(T, B, 2, DECAY_LORA)
    al = al.reshape(T, B, 2, AAA_LORA)
    w_log = -jax.nn.softplus(-(w0 + jnp.einsum('tbdl,dlc->tbdc', jnp.tanh(wl), w_up))) - 0.5
    decay = jnp.exp(-jnp.exp(w_log))
    a = jax.nn.sigmoid(a0 + jnp.einsum('tbdl,dlc->tbdc', al, a_up))
    g = jax.nn.sigmoid(gl) @ g_up
    kk = l2norm((k * k_k).reshape(T, B, HA, NA))
    k_dir = k[:, :, None] * (1.0 + (a - 1.0) * k_a)
    b_dir = kk.reshape(T, B, 1, DA) * a
    heads = lambda t: t.reshape(t.shape[:-1] + (HA, NA))
    return heads(r), heads(v), kk, heads(decay), heads(k_dir), heads(b_dir), g


def wkv7_scan(r, decay, k, v, kk, b, s0, reverse):
    def step(S, inp):
        r_t, w_t, k_t, v_t, kk_t, b_t = inp
        sa = -jnp.einsum('bhvk,bhk->bhv', S, kk_t)
        S = S * w_t[:, :, None, :] + sa[..., None] * b_t[:, :, None, :] + v_t[..., None] * k_t[:, :, None, :]
        return S, jnp.einsum('bhvk,bhk->bhv', S, r_t)
    return lax.scan(step, s0, (r, decay, k, v, kk, b), reverse=reverse)


def rwkv7_mixer(p_ctx, p_lat, mu, w0, w_up, a0, a_up, g_up, k_k, k_a, r_k, lnx_w, lnx_b):
    args = (mu, w0, w_up, a0, a_up, g_up, k_k, k_a)
    ctx_t = rwkv7_prepare(p_ctx, shift_seq(p_ctx), *args)
    lat_t = rwkv7_prepare(p_lat, shift_grid(p_lat), *args)
    zero = jnp.zeros((p_lat.shape[0], HA, NA, NA), jnp.float32)

    def run(t, s_f, s_b):
        r, v, kk, decay, k_dir, b_dir, g = t
        T, B = r.shape[:2]
        s_f, y_f = wkv7_scan(r, decay[:, :, 0], k_dir[:, :, 0], v, kk, b_dir[:, :, 0], s_f, False)
        s_b, y_b = wkv7_scan(r, decay[:, :, 1], k_dir[:, :, 1], v, kk, b_dir[:, :, 1], s_b, True)
        y = y_f + y_b
        m = jnp.mean(y, axis=-1, keepdims=True)
        var = jnp.mean(jnp.square(y - m), axis=-1, keepdims=True)
        y = ((y - m) * lax.rsqrt(var + RWKV_EPS)).reshape(T, B, DA) * lnx_w + lnx_b
        bonus = jnp.sum(r[:, :, None] * k_dir * r_k, axis=(2, 4))
        y = (y + (bonus[..., None] * v).reshape(T, B, DA)) * g
        return jnp.swapaxes(y, 0, 1), s_f, s_b

    y_ctx, s_f, s_b = run(ctx_t, zero, zero)
    y_lat, _, _ = run(lat_t, s_f, s_b)
    return y_ctx, y_lat


def dwconv_centered(x, w):
    C = x.shape[-1]
    return lax.conv_general_dilated(
        x, w[:, None, :].astype(x.dtype), window_strides=(1,),
        padding=[(GDN_CONV // 2, GDN_CONV // 2)],
        dimension_numbers=('NWC', 'WIO', 'NWC'), feature_group_count=C)


def gdn_prepare(p, conv_w, A_log, dt_bias):
    B, T, _ = p.shape
    qkv = jax.nn.silu(dwconv_centered(p[..., :3 * DB], conv_w)).astype(jnp.float32)
    q, k, v = jnp.split(qkv, 3, axis=-1)
    q = l2norm(q.reshape(B, T, HB, KB)) * (KB ** -0.5)
    k = l2norm(k.reshape(B, T, HB, KB))
    v = v.reshape(B, T, HB, KB)
    z = p[..., 3 * DB:4 * DB]
    gl = p[..., 4 * DB:].astype(jnp.float32).reshape(B, T, 2, 2, HB)
    g = -jnp.exp(A_log) * jax.nn.softplus(gl[:, :, 0] + dt_bias)
    beta = jax.nn.sigmoid(gl[:, :, 1])
    return q, k, v, g, beta, z


def gdn_chunked(q, k, v, g, beta, s0):
    B, T, H, K = q.shape
    V = v.shape[-1]
    C = GDN_CHUNK
    n = T // C
    blk = lambda t: jnp.moveaxis(t.reshape((B, n, C, H) + t.shape[3:]), 3, 1)
    q, k, v, g, beta = (blk(t) for t in (q, k, v, g, beta))
    gc = jnp.cumsum(g, axis=-1)
    idx = jnp.arange(C)
    incl = idx[:, None] >= idx[None, :]
    strict = idx[:, None] > idx[None, :]
    decay = jnp.where(incl, jnp.exp(jnp.where(incl, gc[..., :, None] - gc[..., None, :], 0.0)), 0.0)
    kb = k * beta[..., None]
    a_low = jnp.where(strict, jnp.einsum('bhnik,bhnjk->bhnij', kb, k) * decay, 0.0)
    rhs = jnp.concatenate([v * beta[..., None], kb * jnp.exp(gc)[..., None]], axis=-1)
    sol = lax.linalg.triangular_solve(a_low + jnp.eye(C, dtype=jnp.float32), rhs,
                                      left_side=True, lower=True, unit_diagonal=True)
    u, w = sol[..., :V], sol[..., V:]
    attn = jnp.einsum('bhnik,bhnjk->bhnij', q, k) * decay
    qg = q * jnp.exp(gc)[..., None]
    kg = k * jnp.exp(gc[..., -1:] - gc)[..., None]
    g_last = jnp.exp(gc[..., -1])

    def step(S, inp):
        u_i, w_i, a_i, qg_i, kg_i, gl_i = inp
        v_new = u_i - jnp.einsum('bhck,bhkv->bhcv', w_i, S)
        o = jnp.einsum('bhck,bhkv->bhcv', qg_i, S) + jnp.einsum('bhcs,bhsv->bhcv', a_i, v_new)
        S = S * gl_i[..., None, None] + jnp.einsum('bhck,bhcv->bhkv', kg_i, v_new)
        return S, o

    xs = tuple(jnp.moveaxis(t, 2, 0) for t in (u, w, attn, qg, kg, g_last))
    S, o = lax.scan(step, s0, xs)
    o = jnp.transpose(o, (1, 0, 3, 2, 4)).reshape(B, T, H, V)
    return S, o


def gdn_mixer(p_ctx, p_lat, conv_w, A_log, dt_bias, onorm_g):
    ctx_t = gdn_prepare(p_ctx, conv_w, A_log, dt_bias)
    lat_t = gdn_prepare(p_lat, conv_w, A_log, dt_bias)
    zero = jnp.zeros((p_lat.shape[0], HB, KB, KB), jnp.float32)
    flip = lambda t: jnp.flip(t, axis=1)

    def run(t, s_f, s_b):
        q, k, v, g, beta, z = t
        B, T = q.shape[:2]
        s_f, o_f = gdn_chunked(q, k, v, g[:, :, 0], beta[:, :, 0], s_f)
        s_b, o_b = gdn_chunked(flip(q), flip(k), flip(v), flip(g[:, :, 1]), flip(beta[:, :, 1]), s_b)
        o = o_f + flip(o_b)
        o = o * lax.rsqrt(jnp.mean(o * o, axis=-1, keepdims=True) + NORM_EPS) * onorm_g
        return o.reshape(B, T, DB) * jax.nn.silu(z.astype(jnp.float32)), s_f, s_b

    y_ctx, s_f, s_b = run(ctx_t, zero, zero)
    y_lat, _, _ = run(lat_t, s_f, s_b)
    return y_ctx, y_lat


def moe_ffn(h, router_w, router_b, w1, w3, w2, sw1, sw3, sw2):
    B, T, D = h.shape
    N = B * T
    xs = h.reshape(N, D)
    scores = jax.nn.sigmoid((xs @ router_w).astype(jnp.float32))
    sel = scores + router_b.astype(jnp.float32)
    grp_score = jnp.sum(lax.top_k(sel.reshape(N, N_GROUPS, N_EXPERTS // N_GROUPS), 2)[0], axis=-1)
    top_g = lax.top_k(grp_score, TOPK_GROUPS)[1]
    gmask = jnp.any(top_g[:, :, None] == jnp.arange(N_GROUPS)[None, None, :], axis=1)
    emask = jnp.repeat(gmask, N_EXPERTS // N_GROUPS, axis=1)
    top_e = lax.top_k(jnp.where(emask, sel, -jnp.inf), TOP_K)[1]
    wts = jnp.take_along_axis(scores, top_e, axis=1)
    wts = wts / jnp.sum(wts, axis=-1, keepdims=True) * ROUTED_SCALE

    nk = N * TOP_K
    e_flat = top_e.reshape(nk)
    order = jnp.argsort(e_flat)
    e_sorted = e_flat[order]
    tok_sorted = (jnp.arange(nk, dtype=jnp.int32) // TOP_K)[order]
    w_sorted = wts.reshape(nk)[order]
    cnt = jnp.bincount(e_flat, length=N_EXPERTS)
    padded = (cnt + MOE_BLOCK - 1) // MOE_BLOCK * MOE_BLOCK
    pend = jnp.cumsum(padded)
    dest = (pend - padded)[e_sorted] + jnp.arange(nk, dtype=jnp.int32) - (jnp.cumsum(cnt) - cnt)[e_sorted]
    n_blk = (nk + N_EXPERTS * (MOE_BLOCK - 1) + MOE_BLOCK - 1) // MOE_BLOCK
    buf_tok = jnp.full((n_blk * MOE_BLOCK,), N, jnp.int32).at[dest].set(tok_sorted)
    buf_w = jnp.zeros((n_blk * MOE_BLOCK,), h.dtype).at[dest].set(w_sorted.astype(h.dtype))
    blk_e = jnp.minimum(jnp.searchsorted(pend, jnp.arange(n_blk, dtype=jnp.int32) * MOE_BLOCK, side='right'),
                        N_EXPERTS - 1)
    x_pad = jnp.concatenate([xs, jnp.zeros((1, D), xs.dtype)], axis=0)

    def block(acc, inp):
        tok, wt, e = inp
        xb = x_pad[tok]
        yb = (jax.nn.silu(xb @ w1[e]) * (xb @ w3[e])) @ w2[e]
        return acc.at[tok].add(yb * wt[:, None]), None

    acc, _ = lax.scan(block, jnp.zeros((N + 1, D), h.dtype),
                      (buf_tok.reshape(n_blk, MOE_BLOCK), buf_w.reshape(n_blk, MOE_BLOCK), blk_e))
    shared = (jax.nn.silu(xs @ sw1) * (xs @ sw3)) @ sw2
    return (acc[:N] + shared).reshape(B, T, D)


def setup_inputs(seed: int = 0) -> dict:
    key = jax.random.key(seed)
    ks = iter(jax.random.split(key, 48))
    nrm = lambda shape, s: jax.random.normal(next(ks), shape, jnp.float32) * s
    unif = lambda shape, lo, hi: jax.random.uniform(next(ks), shape, jnp.float32, lo, hi)
    L, D = DEPTH, D_MODEL
    dt = unif((L, 2, HB), 1e-3, 1e-1)
    return {
        'x': nrm((BATCH, SEQ, D), 1.0),
        'c': nrm((BATCH, D), 1.0),
        'ctx': nrm((BATCH, CTX_LEN, D), 1.0),
        'c_ctx': nrm((D,), 1.0),
        'w_ada': nrm((L, D, 6 * D), 0.5 * D ** -0.5),
        'b_ada': nrm((L, 6 * D), 0.02),
        'norm1_g': 1.0 + nrm((L, D), 0.02),
        'w_in': nrm((L, D, IN_COLS), D ** -0.5),
        'mu_shift': unif((L, RWKV_COLS), 0.0, 1.0),
        'w0': unif((L, 2, DA), -6.0, -1.0),
        'w_up': nrm((L, 2, DECAY_LORA, DA), 0.1),
        'a0': nrm((L, 2, DA), 0.1),
        'a_up': nrm((L, 2, AAA_LORA, DA), 0.1),
        'g_up': nrm((L, GATE_LORA, DA), GATE_LORA ** -0.5),
        'k_k': 0.85 + nrm((L, DA), 0.05),
        'k_a': 1.0 + nrm((L, DA), 0.05),
        'r_k': nrm((L, HA, NA), 0.1),
        'lnx_w': 1.0 + nrm((L, DA), 0.02),
        'lnx_b': nrm((L, DA), 0.02),
        'conv_w': nrm((L, GDN_CONV, 3 * DB), GDN_CONV ** -0.5),
        'A_log': jnp.log(unif((L, 2, HB), 1.0, 16.0)),
        'dt_bias': dt + jnp.log(-jnp.expm1(-dt)),
        'onorm_g': 1.0 + nrm((L, KB), 0.02),
        'w_out': nrm((L, D_MIX, D), D_MIX ** -0.5),
        'norm2_g': 1.0 + nrm((L, D), 0.02),
        'router_w': nrm((L, D, N_EXPERTS), D ** -0.5),
        'router_b': nrm((L, N_EXPERTS), 0.01),
        'exp_w1': nrm((L, N_EXPERTS, D, D_EXPERT), D ** -0.5),
        'exp_w3': nrm((L, N_EXPERTS, D, D_EXPERT), D ** -0.5),
        'exp_w2': nrm((L, N_EXPERTS, D_EXPERT, D), D_EXPERT ** -0.5),
        'sh_w1': nrm((L, D, D_SHARED), D ** -0.5),
        'sh_w3': nrm((L, D, D_SHARED), D ** -0.5),
        'sh_w2': nrm((L, D_SHARED, D), D_SHARED ** -0.5),
        'final_g': 1.0 + nrm((D,), 0.02),
    }


def reference(x, c, ctx, c_ctx, w_ada, b_ada, norm1_g, w_in, mu_shift, w0, w_up, a0, a_up, g_up,
              k_k, k_a, r_k, lnx_w, lnx_b, conv_w, A_log, dt_bias, onorm_g, w_out, norm2_g,
              router_w, router_b, exp_w1, exp_w3, exp_w2, sh_w1, sh_w3, sh_w2, final_g):
    h_x, h_c = x, ctx
    for l in range(DEPTH):
        mod = jax.nn.silu(c) @ w_ada[l] + b_ada[l]
        mod_c = jax.nn.silu(c_ctx) @ w_ada[l] + b_ada[l]
        sh1, sc1, gt1, sh2, sc2, gt2 = (m[:, None] for m in jnp.split(mod, 6, axis=-1))
        sh1c, sc1c, gt1c, sh2c, sc2c, gt2c = jnp.split(mod_c, 6, axis=-1)

        p_x = (rmsnorm(h_x, norm1_g[l]) * (1.0 + sc1) + sh1) @ w_in[l]
        p_c = (rmsnorm(h_c, norm1_g[l]) * (1.0 + sc1c) + sh1c) @ w_in[l]
        ya_c, ya_x = rwkv7_mixer(p_c[..., :RWKV_COLS], p_x[..., :RWKV_COLS], mu_shift[l], w0[l], w_up[l],
                                 a0[l], a_up[l], g_up[l], k_k[l], k_a[l], r_k[l], lnx_w[l], lnx_b[l])
        yb_c, yb_x = gdn_mixer(p_c[..., RWKV_COLS:], p_x[..., RWKV_COLS:], conv_w[l], A_log[l],
                               dt_bias[l], onorm_g[l])
        h_x = h_x + gt1 * (jnp.concatenate([ya_x, yb_x], axis=-1).astype(x.dtype) @ w_out[l])

        h_x = h_x + gt2 * moe_ffn(rmsnorm(h_x, norm2_g[l]) * (1.0 + sc2) + sh2, router_w[l], router_b[l],
                                  exp_w1[l], exp_w3[l], exp_w2[l], sh_w1[l], sh_w3[l], sh_w2[l])

        if l < DEPTH - 1:
            h_c = h_c + gt1c * (jnp.concatenate([ya_c, yb_c], axis=-1).astype(x.dtype) @ w_out[l])
            h_c = h_c + gt2c * moe_ffn(rmsnorm(h_c, norm2_g[l]) * (1.0 + sc2c) + sh2c, router_w[l], router_b[l],
                                       exp_w1[l], exp_w3[l], exp_w2[l], sh_w1[l], sh_w3[l], sh_w2[l])
    return rmsnorm(h_x, final_g)
```

```python
import contextlib
import os
import numpy as np
import concourse.bass as bass
import concourse.mybir as mybir
from concourse.bass_utils import run_bass_kernel_spmd

F32 = mybir.dt.float32
BF16 = mybir.dt.bfloat16
I32 = mybir.dt.int32
AF = mybir.ActivationFunctionType
ALU = mybir.AluOpType
AX = mybir.AxisListType

EPOCH = 60000
D = 1024
KAPPA = 0.6065306597126334


class Prog:
    def __init__(self, nc, n_dma_sems=48):
        self.nc = nc
        self.es = contextlib.ExitStack()
        self.eng = {"pe": nc.tensor, "dve": nc.vector, "act": nc.scalar,
                    "pool": nc.gpsimd, "sp": nc.sync}
        self.cnt = {e: 0 for e in self.eng}
        self.esem = {}
        self.nsem = 0
        for e in self.eng:
            self.esem[e] = self._newsem(f"e_{e}")
        self.dsems = [[self._newsem(f"d{i}"), 0] for i in range(n_dma_sems)]
        self.dnext = 0
        self.synced = {e: {} for e in self.eng}
        self.lastw = {}
        self.readers = {}
        self.ninstr = {e: 0 for e in self.eng}
        self.nwait = 0
        self.ndma = 0

    def _newsem(self, name):
        self.nsem += 1
        sem = self.es.enter_context(self.nc.semaphore(f"{name}_{self.nsem}"))
        if not hasattr(self, "allsems"):
            self.allsems = []
        self.allsems.append(sem)
        return sem

    def sb(self, name, shape, dt=F32, es=None):
        self.nsem += 1
        return (es or self.es).enter_context(self.nc.sbuf_tensor(f"{name}_u{self.nsem}", list(shape), dt))

    def ps(self, name, shape, dt=F32):
        return self.es.enter_context(self.nc.psum_tensor(name, list(shape), dt))

    def _wait(self, eng, ev, raw=True):
        if ev is None:
            return
        sem, val, src = ev
        if src == eng:
            if eng == "pe":
                return
        sy = self.synced[eng]
        key = id(sem)
        if sy.get(key, 0) >= val:
            return
        self.eng[eng].wait_ge(sem, val)
        self.nwait += 1
        sy[key] = val

    def _deps(self, eng, reads, writes):
        for k in reads:
            self._wait(eng, self.lastw.get(k))
        for k in writes:
            self._wait(eng, self.lastw.get(k), raw=False)
            for ev in self.readers.get(k, {}).values():
                self._wait(eng, ev, raw=False)

    def _record(self, ev, reads, writes):
        for k in reads:
            self.readers.setdefault(k, {})[id(ev[0])] = ev
        for k in writes:
            self.lastw[k] = ev
            self.readers[k] = {}

    def _limited(self):
        import os
        lim = int(os.environ.get("BASS_LIMIT", "0"))
        self.total = getattr(self, "total", 0) + 1
        return lim > 0 and self.total > lim

    def op(self, eng, fn, reads=(), writes=()):
        if self._limited():
            return None
        self._deps(eng, reads, writes)
        if self.cnt[eng] >= EPOCH:
            if not hasattr(self, "tail_ev"):
                self.tail_ev = {}
            self.tail_ev[eng] = (self.esem[eng], self.cnt[eng], eng)
            self.esem[eng] = self._newsem(f"e_{eng}")
            self.cnt[eng] = 0
        ins = fn()
        self.cnt[eng] += 1
        ins.then_inc(self.esem[eng], 1)
        ev = (self.esem[eng], self.cnt[eng], eng)
        self._record(ev, reads, writes)
        self.ninstr[eng] += 1
        return ev

    def dma(self, q, out, in_, reads=(), writes=(), indirect=None, force=False, **kw):
        if not force and self._limited():
            return None
        self._deps(q, reads, writes)
        slot = self.dsems[self.dnext]
        self.dnext = (self.dnext + 1) % len(self.dsems)
        sem, val = slot
        if val > 0:
            self._wait(q, (sem, val, "dma"))
        if val + 16 > 60000:
            raise RuntimeError("dma semaphore overflow")
        slot[1] = val + 16
        if indirect is None:
            ins = self.eng[q].dma_start(out=out, in_=in_, **kw)
        else:
            ins = self.eng[q].indirect_dma_start(out=out, in_=in_, **indirect, **kw)
        ins.then_inc(sem, 16)
        ev = (sem, val + 16, "dma")
        self._record(ev, reads, writes)
        self.ndma += 1
        return ev

    def barrier(self):
        evs = [(self.esem[e], self.cnt[e], e) for e in self.eng if self.cnt[e] > 0]
        evs += list(getattr(self, "tail_ev", {}).values())
        devs = [(s, v, "dma") for s, v in self.dsems if v > 0]
        for e in self.eng:
            for ev in evs:
                self._wait(e, ev)
            for ev in devs:
                self._wait(e, ev)
        self.lastw = {}
        self.readers = {}

    def finish(self):
        devs = [(s, v, "dma") for s, v in self.dsems if v > 0]
        for ev in devs:
            self._wait("sp", ev)
        for e in self.eng:
            if e != "sp" and self.cnt[e] > 0:
                self._wait("sp", (self.esem[e], self.cnt[e], e))

    def _pe_rowgroup(self, src, writes):
        lo = src.base_partition()
        hi = lo + src.partition_size()
        if not hasattr(self, "pe_last"):
            self.pe_last = {}
        for k in writes:
            prev = self.pe_last.get(k)
            if prev is not None and (prev[1] <= lo or hi <= prev[0]):
                sem, val, _ = prev[2]
                if self.synced["pe"].get(id(sem), 0) < val:
                    self.eng["pe"].wait_ge(sem, val)
                    self.nwait += 1
                    self.synced["pe"][id(sem)] = val
        return lo, hi

    def mm(self, out, lhsT, rhs, start=True, stop=True, reads=(), writes=()):
        lo, hi = self._pe_rowgroup(lhsT, writes)
        ev = self.op("pe", lambda: self.nc.tensor.matmul(out, lhsT, rhs, start=start, stop=stop),
                     reads, writes)
        if ev is not None:
            for k in writes:
                self.pe_last[k] = (lo, hi, ev)
        return ev

    def tr(self, out, in_, ident, reads=(), writes=()):
        lo, hi = self._pe_rowgroup(in_, writes)
        ev = self.op("pe", lambda: self.nc.tensor.transpose(out, in_, ident), reads, writes)
        if ev is not None:
            for k in writes:
                self.pe_last[k] = (lo, hi, ev)
        return ev

    def act(self, out, in_, func, bias=None, scale=None, accum_out=None, reads=(), writes=()):
        kw = {}
        if bias is not None:
            kw["bias"] = bias
        if scale is not None:
            kw["scale"] = scale
        if accum_out is not None:
            kw["accum_out"] = accum_out
        return self.op("act", lambda: self.nc.scalar.activation(out=out, in_=in_, func=func, **kw),
                       reads, writes)

    def ts(self, eng, out, in0, s1, op0, s2=None, op1=None, accum_out=None, reads=(), writes=()):
        e = self.eng[eng]
        kw = {}
        if op1 is not None:
            kw["op1"] = op1
        if accum_out is not None:
            kw["accum_out"] = accum_out
        return self.op(eng, lambda: e.tensor_scalar(out=out, in0=in0, scalar1=s1, scalar2=s2, op0=op0, **kw),
                       reads, writes)

    def tt(self, eng, out, in0, in1, op, reads=(), writes=()):
        e = self.eng[eng]
        return self.op(eng, lambda: e.tensor_tensor(out=out, in0=in0, in1=in1, op=op), reads, writes)

    def stt(self, out, in0, scalar, in1, op0, op1, accum_out=None, reads=(), writes=()):
        kw = {}
        if accum_out is not None:
            kw["accum_out"] = accum_out
        return self.op("dve", lambda: self.nc.vector.scalar_tensor_tensor(
            out=out, in0=in0, scalar=scalar, in1=in1, op0=op0, op1=op1, **kw), reads, writes)

    def copy(self, eng, out, in_, reads=(), writes=()):
        if eng == "act":
            return self.op("act", lambda: self.nc.scalar.copy(out=out, in_=in_), reads, writes)
        e = self.eng[eng]
        return self.op(eng, lambda: e.tensor_copy(out=out, in_=in_), reads, writes)

    def memset(self, eng, out, val, reads=(), writes=()):
        e = self.eng[eng]
        return self.op(eng, lambda: e.memset(out, val), reads, writes)

    def aselect(self, out, in_, pattern, cmp, fill, base, cm, reads=(), writes=()):
        if not hasattr(self, "fregs"):
            self.fregs = {}
        if fill not in self.fregs:
            self.fregs[fill] = self.nc.gpsimd.to_reg(float(fill))
        fill = self.fregs[fill]
        return self.op("pool", lambda: self.nc.gpsimd.affine_select(
            out=out, in_=in_, pattern=pattern, compare_op=cmp, fill=fill, base=base,
            channel_multiplier=cm), reads, writes)


class Cfg:
    def __init__(self, NB=4, ROWS=32, CTX=256, CAP=768, stop=None, debug=()):
        self.NB = NB
        self.ROWS = ROWS
        self.CTX = CTX
        self.TL = ROWS * 64
        self.T = self.CTX + self.TL
        self.NCH = self.T // 64
        self.NCC = self.CTX // 64
        self.NTT = self.T // 128
        self.NTC = self.CTX // 128
        self.NTL = self.TL // 128
        self.NBP = NB + 1
        self.CAP = CAP
        self.stop = stop
        self.debug = tuple(debug)


class Builder:
    def __init__(self, cfg):
        self.cfg = cfg
        self.nc = bass.Bass("TRN2", target_bir_lowering=False)
        self.P = Prog(self.nc)
        self.din = {}
        self.dout = {}
        self.dbg = {}

    def inp(self, name, shape, dt=F32):
        t = self.nc.dram_tensor(name, list(shape), dt, kind="ExternalInput").ap()
        self.din[name] = t
        return t

    def outp(self, name, shape, dt=F32):
        t = self.nc.dram_tensor(name, list(shape), dt, kind="ExternalOutput").ap()
        self.dout[name] = t
        return t

    def scratch(self, name, shape, dt=F32):
        return self.nc.dram_tensor(name, list(shape), dt).ap()

    def dump(self, name, ap_sb, shape, keys, dt=F32):
        if name not in self.cfg.debug:
            return
        o = self.outp("dbg_" + name, shape, dt)
        self.P.dma("sp", o, ap_sb, reads=keys, writes=["dbg_" + name])

    def declare(self):
        c = self.cfg
        i = self.inp
        self.cT = i("cT", [128, 8, c.NBP])
        self.w_ada = i("w_ada", [1024, 6144])
        self.b_ada_rep = i("b_ada_rep", [c.NBP, 6144])
        self.g1_rep = i("g1_rep", [c.NBP, 1024])
        self.g2_rep = i("g2_rep", [c.NBP, 1024])
        self.x = i("x", [c.NB, c.TL, 1024])
        self.ctx = i("ctx", [c.NB, c.CTX, 1024])
        self.w_in_r = i("w_in_r", [1024, 1920])
        self.muT_d = i("muT", [128, 15])
        self.dmask_d = i("dmask", [128, 6, 15])
        self.w0T_d = i("w0T", [128, 2, 4])
        self.a0T_d = i("a0T", [128, 2, 4])
        self.kkT_d = i("kkT", [128, 4])
        self.kaT_d = i("kaT", [128, 4])
        self.rkT_d = i("rkT", [128, 4])
        self.w_up_d = i("w_up", [128, 512])
        self.a_up_d = i("a_up", [128, 512])
        self.g_up_d = i("g_up", [128, 512])
        self.lnxw_d = i("lnxw_rep", [128, 512])
        self.lnxb_d = i("lnxb_rep", [128, 512])
        self.gdn_declare()
        self.post_declare()

    def consts(self):
        P, nc = self.P, self.nc
        self.identf = P.sb("identf", [128, 128], F32)
        self.identb = P.sb("identb", [128, 128], BF16)
        P.memset("pool", self.identf[:], 0.0, writes=["identf"])
        P.aselect(self.identf[:], self.identf[:], [[-1, 128]], ALU.not_equal, 1.0, 0, 1,
                  reads=["identf"], writes=["identf"])
        P.copy("dve", self.identb[:], self.identf[:], reads=["identf"], writes=["identb"])
        self.bd = P.sb("bdones", [128, 128], BF16)
        P.memset("dve", self.bd[:], 0.0, writes=["bd"])
        P.memset("dve", self.bd[0:64, 0:64], 1.0, writes=["bd"])
        P.memset("dve", self.bd[64:128, 64:128], 1.0, writes=["bd"])
        self.rmask = []
        for d in range(2):
            m = P.sb(f"rmask{d}", [128, 128], BF16)
            P.memset("pool", m[:], 1.0, writes=[f"rmask{d}"])
            for ph in range(2):
                for kind in range(2):
                    blk = m[ph * 64:(ph + 1) * 64, kind * 64:(kind + 1) * 64]
                    sgn = 1 if d == 0 else -1
                    P.aselect(blk, blk, [[sgn, 64]], ALU.is_gt if kind == 0 else ALU.is_ge, 0.0, 0, -sgn,
                              reads=[f"rmask{d}"], writes=[f"rmask{d}"])
            self.rmask.append(m)
        self.rst = P.sb("rst", [128, 512], F32)
        P.memset("dve", self.rst[:], 1.0, writes=["rst"])
        P.memset("dve", self.rst[:].rearrange("p (a c) -> p a c", c=64)[:, :, 0:1], 0.0, writes=["rst"])
        self.one1 = P.sb("one1", [128, 1], F32)
        P.memset("dve", self.one1[:], 1.0, writes=["one1"])
        self.eps6 = P.sb("eps6", [128, 1], F32)
        P.memset("dve", self.eps6[:], 1e-6, writes=["eps6"])
        self.pb = [P.ps(f"pb{i}", [128, 512], F32) for i in range(8)]

    def pbT(self, i):
        return self.pb[i][:].bitcast(BF16)

    def phase0(self):
        c, P, nc = self.cfg, self.P, self.nc
        NBP = c.NBP
        self.A1T = P.sb("A1T", [128, 8, NBP])
        self.B1T = P.sb("B1T", [128, 8, NBP])
        self.modscr = self.scratch("modscr", [NBP, 4, 1024])
        with contextlib.ExitStack() as es:
            modrow = P.sb("modrow", [NBP, 6144], es=es)
            cT = P.sb("cT_sb", [128, 8, NBP], es=es)
            brep = P.sb("brep", [NBP, 6144], es=es)
            g1r = P.sb("g1r", [NBP, 1024], es=es)
            g2r = P.sb("g2r", [NBP, 1024], es=es)
            wst = [P.sb(f"wada{j}", [128, 8, 512], es=es) for j in range(2)]
            P.dma("sp", cT[:], self.cT, writes=["cT"])
            P.dma("sp", brep[:], self.b_ada_rep, writes=["brep"])
            P.dma("sp", g1r[:], self.g1_rep, writes=["g1r"])
            P.dma("sp", g2r[:], self.g2_rep, writes=["g2r"])
            P.act(cT[:], cT[:], AF.Silu, reads=["cT"], writes=["cT"])
            for j in range(12):
                w = wst[j % 2]
                P.dma("sp", w[:], self.w_ada[:, j * 512:(j + 1) * 512].rearrange("(k p) c -> p k c", p=128),
                      writes=[f"wada{j % 2}"])
                bank = self.pb[1 + j % 2]
                for k in range(8):
                    P.mm(bank[0:NBP, :], cT[:, k, :], w[:, k, :], start=(k == 0), stop=(k == 7),
                         reads=["cT", f"wada{j % 2}"], writes=[f"pb{1 + j % 2}"])
                P.tt("dve", modrow[:, j * 512:(j + 1) * 512], bank[0:NBP, :], brep[:, j * 512:(j + 1) * 512],
                     ALU.add, reads=[f"pb{1 + j % 2}", "brep"], writes=["modrow"])
            blk = lambda i: modrow[:, i * 1024:(i + 1) * 1024]
            P.stt(blk(1), blk(1), 1.0, g1r[:], ALU.add, ALU.mult, reads=["modrow", "g1r"], writes=["modrow"])
            P.stt(blk(4), blk(4), 1.0, g2r[:], ALU.add, ALU.mult, reads=["modrow", "g2r"], writes=["modrow"])
            bank = self.pb[3]
            for k in range(8):
                P.tr(bank[:, k * NBP:(k + 1) * NBP], modrow[:, 1024 + k * 128:1024 + (k + 1) * 128],
                     self.identf[0:NBP, 0:NBP], reads=["modrow", "identf"], writes=["pb3"])
                P.tr(bank[:, (8 + k) * NBP:(9 + k) * NBP], modrow[:, k * 128:(k + 1) * 128],
                     self.identf[0:NBP, 0:NBP], reads=["modrow", "identf"], writes=["pb3"])
            P.copy("dve", self.A1T[:].rearrange("p k b -> p (k b)"), bank[:, 0:8 * NBP], reads=["pb3"], writes=["A1T"])
            P.copy("dve", self.B1T[:].rearrange("p k b -> p (k b)"), bank[:, 8 * NBP:16 * NBP], reads=["pb3"], writes=["B1T"])
            for j, bi in enumerate((2, 4, 3, 5)):
                P.dma("sp", self.modscr[:, j, :], blk(bi), reads=["modrow"], writes=["modscr"])
            self.dump("A1T", self.A1T[:], [128, 8, NBP], ["A1T"])
            self.dump("modrow", modrow[:], [NBP, 6144], ["modrow"])
            P.barrier()

    def phase1(self, b, h1T, es):
        c, P, nc = self.cfg, self.P, self.nc
        xt = [P.sb(f"xt{j}", [128, 1024], es=es) for j in range(2)]
        xn = P.sb("xn", [128, 1024], BF16, es=es)
        junk = P.sb("junk", [128, 1024], BF16, es=es)
        ss = P.sb("ss", [128, 1], es=es)
        tmpf = P.sb("h1tmp", [128, 8, 128], es=es)
        for tt in range(c.NTT):
            xb = xt[tt % 2]
            kx = f"xt{tt % 2}"
            if tt < c.NTC:
                src = self.ctx[b, tt * 128:(tt + 1) * 128, :]
                col = c.NB
            else:
                src = self.x[b, (tt - c.NTC) * 128:(tt - c.NTC + 1) * 128, :]
                col = b
            P.dma("sp", xb[:], src, writes=[kx])
            P.act(junk[:], xb[:], AF.Square, accum_out=ss[:], reads=[kx], writes=["junk", "ss"])
            P.ts("dve", ss[:], ss[:], 1.0 / 1024, ALU.mult, 1e-6, ALU.add, reads=["ss"], writes=["ss"])
            P.act(ss[:], ss[:], AF.Sqrt, reads=["ss"], writes=["ss"])
            P.op("dve", lambda: nc.vector.reciprocal(out=ss[:], in_=ss[:]), reads=["ss"], writes=["ss"])
            P.act(xn[:], xb[:], AF.Copy, scale=ss[:, 0:1], reads=[kx, "ss"], writes=["xn"])
            bT = self.pbT(5)
            for k in range(8):
                P.tr(bT[:, k * 128:(k + 1) * 128], xn[:, k * 128:(k + 1) * 128], self.identb[:],
                     reads=["xn", "identb"], writes=["pb5"])
            P.tt("dve", tmpf[:], bT.rearrange("p (k t) -> p k t", k=8),
                 self.A1T[:, :, col:col + 1].to_broadcast([128, 8, 128]), ALU.mult,
                 reads=["pb5", "A1T"], writes=["h1tmp"])
            P.tt("pool", h1T[:, :, tt * 128:(tt + 1) * 128], tmpf[:],
                 self.B1T[:, :, col:col + 1].to_broadcast([128, 8, 128]), ALU.add,
                 reads=["h1tmp", "B1T"], writes=["h1T"])

    def inproj(self, h1T, w_dram, ntiles, pT, pkey, es):
        c, P, nc = self.cfg, self.P, self.nc
        wst = [P.sb(f"wst{j}", [128, 8, 128], es=es) for j in range(2)]
        wbf = [P.sb(f"wbf{j}", [128, 8, 128], BF16, es=es) for j in range(2)]
        nts = []
        t = 0
        while t < c.T:
            w = min(512, c.T - t)
            nts.append((t, w))
            t += w
        ev = 0
        for i in range(ntiles):
            j = i % 2
            P.dma("sp", wst[j][:], w_dram[:, i * 128:(i + 1) * 128].rearrange("(k p) c -> p k c", p=128),
                  writes=[f"wst{j}"])
            P.copy("pool", wbf[j][:], wst[j][:], reads=[f"wst{j}"], writes=[f"wbf{j}"])
            for (t0, w) in nts:
                bi = 1 + ev % 4
                bank = self.pb[bi]
                for k in range(8):
                    P.mm(bank[:, 0:w], wbf[j][:, k, :], h1T[:, k, t0:t0 + w], start=(k == 0), stop=(k == 7),
                         reads=[f"wbf{j}", "h1T"], writes=[f"pb{bi}"])
                P.copy("act" if ev % 2 == 0 else "dve", pT[:, i, t0:t0 + w], bank[:, 0:w],
                       reads=[f"pb{bi}"], writes=[pkey])
                ev += 1

    def build(self):
        c, P, nc = self.cfg, self.P, self.nc
        self.declare()
        with P.es:
            self.consts()
            self.post_setup()
            self.phase0()
            if c.stop == "phase0":
                P.finish()
                return
            blist = list(range(min(c.NB, int(os.environ.get("KB_BATCHES", "99")))))
            if os.environ.get("KB_ONLY"):
                blist = [int(v) for v in os.environ["KB_ONLY"].split(",")]
            for b in blist:
                self.batch(b)
                if c.stop in ("pT", "rwkv", "gdn"):
                    break
            skip = os.environ.get("KB_SKIP", "")
            if c.stop is None:
                if "moe" not in skip:
                    self.moe()
                if "final" not in skip:
                    self.final()
            if "ymix" in c.debug:
                with contextlib.ExitStack() as esd:
                    fb = P.sb("dbgyb", [128, c.NTL, 1024], BF16, es=esd)
                    f = P.sb("dbgyf", [128, c.NTL, 1024], F32, es=esd)
                    if "skip_rwkv" in c.debug:
                        P.memset("dve", fb[:], 0.0, writes=["dbgyb"])
                        P.dma("sp", fb[:, :, 512:1024], self.ymix_dram[0].rearrange("(n p) c -> p n c", p=128)[:, :, 512:1024], reads=["ymix_dram"], writes=["dbgyb"])
                    else:
                        P.dma("sp", fb[:], self.ymix_dram[0].rearrange("(n p) c -> p n c", p=128), reads=["ymix_dram"], writes=["dbgyb"])
                    P.copy("dve", f[:], fb[:], reads=["dbgyb"], writes=["dbgyf"])
                    self.dump("ymix", f[:], [128, c.NTL, 1024], ["dbgyf"])
                    P.barrier()
            P.finish()

    def batch(self, b):
        c, P, nc = self.cfg, self.P, self.nc
        with contextlib.ExitStack() as esb:
            pT = P.sb("pT", [128, 15, c.T], BF16, es=esb)
            with contextlib.ExitStack() as es1:
                h1T = P.sb("h1T", [128, 8, c.T], BF16, es=es1)
                self.phase1(b, h1T, es1)
                self.inproj(h1T, self.w_in_r, 15, pT, "pT", es1)
                if "h1T" in c.debug:
                    with contextlib.ExitStack() as esd:
                        f = P.sb("dbgf", [128, 8, c.T], F32, es=esd)
                        P.copy("dve", f[:], h1T[:], reads=["h1T"], writes=["dbgf"])
                        self.dump("h1T", f[:], [128, 8, c.T], ["dbgf"])
                        P.barrier()
                P.barrier()
            if "pT" in c.debug:
                with contextlib.ExitStack() as esd:
                    f = P.sb("dbgf", [128, 15, c.T], F32, es=esd)
                    P.copy("dve", f[:], pT[:], reads=["pT"], writes=["dbgf"])
                    self.dump("pT", f[:], [128, 15, c.T], ["dbgf"])
                    P.barrier()
            if c.stop == "pT":
                return
            ph = os.environ.get("KB_PH", "rgp")
            if "skip_rwkv" not in c.debug and "r" in ph:
                self.rwkv(b, pT, esb)
        if c.stop == "rwkv":
            return
        if "g" in ph:
            self.gdn(b)
        if c.stop == "gdn":
            return
        if "p" in ph:
            self.post(b)


def tile_major(v, ntile):
    return np.ascontiguousarray(np.asarray(v, np.float32).reshape(ntile, 128).T)


def prep_shared(inp, cfg):
    f = lambda a: np.ascontiguousarray(np.asarray(a, np.float32))
    NBP = cfg.NBP
    m = {}
    m["w_ada"] = f(inp["w_ada"][0])
    m["b_ada_rep"] = f(np.broadcast_to(inp["b_ada"][0][None, :], (NBP, 6144)))
    m["g1_rep"] = f(np.broadcast_to(inp["norm1_g"][0][None, :], (NBP, 1024)))
    m["g2_rep"] = f(np.broadcast_to(inp["norm2_g"][0][None, :], (NBP, 1024)))
    w_in = np.asarray(inp["w_in"][0], np.float32)
    m["w_in_r"] = f(w_in[:, :1920])
    m["muT"] = tile_major(inp["mu_shift"][0], 15)
    col = np.arange(1920)
    dm = np.zeros((6, 1920), np.float32)
    dm[0] = col < 480
    dm[1] = (col >= 480) & (col < 960)
    dm[2] = (col >= 960) & (col < 1440)
    dm[3] = col >= 1440
    dm[4] = col < 960
    dm[5] = col >= 960
    m["dmask"] = f(np.stack([tile_major(dm[i], 15) for i in range(6)], axis=1))
    pm = lambda a: f(np.asarray(a, np.float32).reshape(2, 4, 128).transpose(2, 0, 1))
    m["w0T"] = pm(inp["w0"][0])
    m["a0T"] = pm(inp["a0"][0])
    m["kkT"] = tile_major(inp["k_k"][0], 4)
    m["kaT"] = tile_major(inp["k_a"][0], 4)
    m["rkT"] = tile_major(np.asarray(inp["r_k"][0]).reshape(512), 4)
    m["w_up"] = f(np.asarray(inp["w_up"][0]).reshape(128, 512))
    m["a_up"] = f(np.asarray(inp["a_up"][0]).reshape(128, 512))
    m["g_up"] = f(inp["g_up"][0])
    m["lnxw_rep"] = f(np.broadcast_to(np.asarray(inp["lnx_w"][0])[None, :], (128, 512)))
    m["lnxb_rep"] = f(np.broadcast_to(np.asarray(inp["lnx_b"][0])[None, :], (128, 512)))
    m["w_in_g"] = f(w_in[:, 1920:1920 + 1536])
    m["w_z"] = f(w_in[:, 1920 + 1536:1920 + 2048])
    wgl = np.zeros((1024, 128), np.float32)
    wgl[:, 0:8] = w_in[:, 3968:3976]
    wgl[:, 32:40] = w_in[:, 3976:3984]
    m["w_gl"] = wgl
    cw = np.asarray(inp["conv_w"][0], np.float32)
    m["cwT"] = f(cw.reshape(5, 12, 128).transpose(2, 1, 0))
    gp = np.zeros((128, 4), np.float32)
    gp[0:8, 0] = np.asarray(inp["dt_bias"][0], np.float32).reshape(8)
    gp[0:8, 1] = np.asarray(inp["A_log"][0], np.float32).reshape(8)
    gp[0:4, 2] = 1.0
    gp[4:8, 3] = 1.0
    m["gpar"] = gp
    W = np.zeros((128, 2, 128), np.float32)
    for dh in range(8):
        W[dh, 0, dh] = 1.0
        W[dh, 0, 8 + dh] = -1.0
        W[dh, 0, 32 + dh] = 1.0
        W[32 + dh, 0, 32 + dh] = 1.0
        W[32 + dh, 0, 96 + dh] = 1.0
        W[dh, 1, 64 + dh] = 1.0
    m["gW"] = W
    gm = np.zeros((128, 2, 2, 64), np.float32)
    jj = np.arange(64)[:, None]
    ii = np.arange(64)[None, :]
    NEG = -30000.0
    gm[0:64, 0, 0] = np.where(ii > jj, 0.0, NEG)
    gm[0:64, 0, 1] = np.where(ii >= jj, 0.0, NEG)
    gm[0:64, 1, 0] = np.where(ii < jj, 0.0, NEG)
    gm[0:64, 1, 1] = np.where(ii <= jj, 0.0, NEG)
    m["gmask"] = gm
    m["onorm_rep"] = f(np.broadcast_to(np.asarray(inp["onorm_g"][0])[None, :], (128, 128)))
    m["w_out"] = f(inp["w_out"][0])
    m["router_w"] = f(inp["router_w"][0])
    m["router_b_rep"] = f(np.broadcast_to(np.asarray(inp["router_b"][0])[None, :], (128, 256)))
    m["ecap"] = f(np.broadcast_to((np.arange(256, dtype=np.float32) * cfg.CAP + 1.0)[None, :], (128, 256)))
    m["sh_w1"] = f(inp["sh_w1"][0]); m["sh_w3"] = f(inp["sh_w3"][0]); m["sh_w2"] = f(inp["sh_w2"][0])
    m["exp_w1"] = f(inp["exp_w1"][0]); m["exp_w3"] = f(inp["exp_w3"][0]); m["exp_w2"] = f(inp["exp_w2"][0])
    m["final_g_rep"] = f(np.broadcast_to(np.asarray(inp["final_g"])[None, :], (128, 1024)))
    return m


def prep_core(inp, cfg, core, shared):
    f = lambda a: np.ascontiguousarray(np.asarray(a, np.float32))
    NB = cfg.NB
    sl = slice(core * NB, (core + 1) * NB)
    m = dict(shared)
    call = np.concatenate([np.asarray(inp["c"], np.float32)[sl], np.asarray(inp["c_ctx"], np.float32)[None, :]], axis=0)
    m["cT"] = f(call.reshape(cfg.NBP, 8, 128).transpose(2, 1, 0))
    m["x"] = f(np.asarray(inp["x"])[sl])
    m["ctx"] = f(np.asarray(inp["ctx"])[sl])
    return m


HORD = (0, 2, 4, 6, 1, 3, 5, 7)


def _rwkv(self, b, pT, esb):
    c, P, nc = self.cfg, self.P, self.nc
    if os.environ.get("BASS_TRACE_TOTAL"):
        print("rwkv enter b", b, "total", getattr(P, "total", 0))
    NTL, NTC, CTX, TL = c.NTL, c.NTC, c.CTX, c.TL
    GS = 1
    W = GS * 64
    NG = c.T // W
    NGC = c.CTX // W
    es = contextlib.ExitStack()
    with es:
        sb = lambda n, s, dt=F32: P.sb(n, s, dt, es=es)
        muT = sb("muT", [128, 15]); omT = sb("omT", [128, 15])
        dmask = sb("dmask", [128, 6, 15]); muD = sb("muD", [128, 6, 15])
        w0T = sb("w0T", [128, 2, 4]); a0T = sb("a0T", [128, 2, 4])
        kkp = sb("kkp", [128, 4]); kap = sb("kap", [128, 4]); omka = sb("omka", [128, 4])
        rkf = sb("rkf", [128, 4]); rkb = sb("rkb", [128, 4], BF16)
        wupf = sb("wupf", [128, 512]); wupb = sb("wupb", [128, 2, 512], BF16)
        aupb = sb("aupb", [128, 2, 512], BF16)
        rkp = sb("rkp", [128, 4, 2], BF16)
        for t_sb, t_d, k in ((muT, self.muT_d, "muT"), (dmask, self.dmask_d, "dmask"), (w0T, self.w0T_d, "w0T"),
                             (a0T, self.a0T_d, "a0T"), (kkp, self.kkT_d, "kkp"), (kap, self.kaT_d, "kap"),
                             (rkf, self.rkT_d, "rkf")):
            P.dma("sp", t_sb[:], t_d, writes=[k])
        for dst, src, k in ((wupb, self.w_up_d, "wupb"), (aupb, self.a_up_d, "aupb")):
            P.memset("pool", dst[:], 0.0, writes=[k])
            P.dma("sp", wupf[:], src, writes=["wupf"])
            for dd in range(2):
                P.copy("dve", dst[dd * 64:(dd + 1) * 64, dd, :], wupf[dd * 64:(dd + 1) * 64, :], reads=["wupf"], writes=[k])
        P.ts("dve", omT[:], muT[:], -1.0, ALU.mult, 1.0, ALU.add, reads=["muT"], writes=["omT"])
        P.tt("dve", muD[:], dmask[:], muT[:].unsqueeze(1).to_broadcast([128, 6, 15]), ALU.mult,
             reads=["dmask", "muT"], writes=["muD"])
        P.ts("dve", omka[:], kap[:], -1.0, ALU.mult, 1.0, ALU.add, reads=["kap"], writes=["omka"])
        P.copy("dve", rkb[:], rkf[:], reads=["rkf"], writes=["rkb"])
        P.memset("pool", rkp[:], 0.0, writes=["rkp"])
        for ee in range(2):
            P.copy("dve", rkp[ee * 64:(ee + 1) * 64, :, ee], rkf[ee * 64:(ee + 1) * 64, :], reads=["rkf"], writes=["rkp"])
        yacc = sb("yacc", [128, NTL, 512], BF16)
        vtok = sb("vtok", [128, NTL, 512], BF16)
        sgT = sb("sgT", [128, TL], BF16)
        bonus = sb("bonus", [128, NTL, 8])
        Sf = sb("Sf", [128, 2, 4, 64]); Sbf = sb("Sbf", [128, 2, 4, 64], BF16)
        P.memset("pool", yacc[:], 0.0, writes=["yacc"])
        P.memset("pool", bonus[:], 0.0, writes=["bonus"])
        P.memset("pool", Sf[:], 0.0, writes=["Sf0", "Sf1"])
        P.memset("pool", Sbf[:], 0.0, writes=["Sbf0", "Sbf1"])
        esp = contextlib.ExitStack()
        sb_outer = sb
        sb = lambda n, s_, dt=F32: P.sb(n, s_, dt, es=esp)
        tmp = {}
        for n in ("xr", "xk", "xv", "kq", "rs", "sig", "aa", "cs", "ce", "rem", "remi",
                  "e_in", "e_ex", "e_ip", "e_rs", "ka", "kd", "t1"):
            tmp[n] = sb("t_" + n, [128, 4, W])
        sqb = sb("sqb", [128, 4, W], BF16)
        prodb = sb("prodb", [128, 4, 2, 64], BF16)
        xw = sb("xw", [128, W]); twl = sb("twl", [128, W], BF16); tal = sb("tal", [128, W], BF16)
        AR, ARp, BKp, BHp, XV, G, Nt, X, Xt, Pa, Pb, RHSb, UV, VZ, BHtok, gC = ([] for _ in range(16))
        BKt = sb("BKt", [128, 4, GS, 2, 64], BF16)
        BHt = sb("BHt", [128, 4, GS, 2, 64], BF16)
        for d in range(2):
            AR.append(sb(f"AR{d}", [128, 4, GS, 2, 64], BF16))
            ARp.append(sb(f"ARp{d}", [128, 2, 4, GS, 2, 64], BF16))
            BKp.append(sb(f"BKp{d}", [128, 2, 4, GS, 2, 64], BF16))
            BHp.append(sb(f"BHp{d}", [128, 2, 4, GS, 2, 64], BF16))
            XV.append(sb(f"XV{d}", [128, 4, GS, 2, 64], BF16))
            VZ.append(sb(f"VZ{d}", [128, 8, 64], BF16))
            for t_, k_ in ((ARp[d], f"ARp{d}"), (BKp[d], f"BKp{d}"), (BHp[d], f"BHp{d}"), (VZ[d], f"VZ{d}")):
                P.memset("pool", t_[:], 0.0, writes=[k_])
            G.append(sb(f"G{d}", [128, 8, 128], BF16))
            Nt.append(sb(f"Nt{d}", [64, 8, 64], BF16))
            X.append(sb(f"X{d}", [64, 8, 64], BF16))
            Xt.append(sb(f"Xt{d}", [64, 8, 64], BF16))
            Pa.append([sb(f"Pa{d}{j}", [64, 8, 64], BF16) for j in range(2)])
            Pb.append([sb(f"Pb{d}{j}", [64, 8, 64], BF16) for j in range(2)])
            RHSb.append(sb(f"RHSb{d}", [64, 8, 64], BF16))
            UV.append(sb(f"UV{d}", [128, 8, 64], BF16))
            BHtok.append(sb(f"BHtok{d}", [128, 8, 128], BF16))
            gC.append(sb(f"gC{d}", [128, 4, GS]))
            P.memset("pool", XV[d][:], 0.0, writes=[f"XV{d}"])
            P.memset("pool", UV[d][:], 0.0, writes=[f"UV{d}"])

        def lerp(dst, dkey, tiles, g):
            t0 = g * W
            isctx = g < NGC
            for j, ti in enumerate(tiles):
                dj = dst[:, j, :] if len(tiles) > 1 else dst
                P.act(dj, pT[:, ti, t0:t0 + W], AF.Copy, scale=omT[:, ti:ti + 1],
                      reads=["pT", "omT"], writes=[dkey])
                cols = range(ti * 128, (ti + 1) * 128)
                if isctx:
                    dirs = [(4, -1)] if cols[-1] < 960 else ([(5, 1)] if cols[0] >= 960 else [(4, -1), (5, 1)])
                else:
                    dirs = []
                    for di, (lo, hi, off) in enumerate(((0, 480, -1), (480, 960, 1), (960, 1440, -64), (1440, 1920, 64))):
                        if cols[0] < hi and cols[-1] >= lo:
                            dirs.append((di, off))
                for (di, off) in dirs:
                    sc = muD[:, di, ti:ti + 1]
                    if isctx:
                        lo = max(t0, 1) if off < 0 else t0
                        hi = t0 + W if off < 0 else min(t0 + W, CTX - 1)
                        if hi <= lo:
                            continue
                        P.stt(dj[:, lo - t0:hi - t0], pT[:, ti, lo + off:hi + off], sc, dj[:, lo - t0:hi - t0],
                              ALU.mult, ALU.add, reads=["pT", "muD", dkey], writes=[dkey])
                    elif abs(off) == 1:
                        dv = dj.rearrange("p (r c) -> p r c", c=64)
                        sv = pT[:, ti, t0:t0 + W].rearrange("p (r c) -> p r c", c=64)
                        if off < 0:
                            o_, s_ = dv[:, :, 1:64], sv[:, :, 0:63]
                        else:
                            o_, s_ = dv[:, :, 0:63], sv[:, :, 1:64]
                        P.stt(o_, s_, sc, o_, ALU.mult, ALU.add, reads=["pT", "muD", dkey], writes=[dkey])
                    else:
                        tau0 = t0 - CTX
                        lo = max(tau0, 64) if off < 0 else tau0
                        hi = tau0 + W if off < 0 else min(tau0 + W, TL - 64)
                        if hi <= lo:
                            continue
                        P.stt(dj[:, lo - tau0:hi - tau0], pT[:, ti, CTX + lo + off:CTX + hi + off], sc,
                              dj[:, lo - tau0:hi - tau0], ALU.mult, ALU.add,
                              reads=["pT", "muD", dkey], writes=[dkey])

        fl = lambda t: t[:].rearrange("p a t -> p (a t)")
        ch = lambda t: t[:].rearrange("p a (c t) -> p (a c) t", t=64)
        slot = lambda t, s: t[:].rearrange("p a c s t -> p (a c) s t")[:, :, s, :]

        def prep(d, g):
            T_ = tmp
            isctx = g < NGC
            c_glob = g
            par = c_glob % 2
            tl = c_glob // 2 - NTC
            lerp(T_["xr"], "xr", [0, 1, 2, 3], g)
            lerp(T_["xk"], "xk", [4, 5, 6, 7], g)
            lerp(T_["xv"], "xv", [8, 9, 10, 11], g)
            lerp(xw[:], "xw", [12], g)
            P.act(twl[:], xw[:], AF.Tanh, reads=["xw"], writes=["twl"])
            lerp(xw[:], "xw", [13], g)
            P.copy("act", tal[:], xw[:], reads=["xw"], writes=["tal"])
            if d == 0 and not isctx:
                lerp(xw[:], "xw", [14], g)
                P.act(sgT[:, g * W - CTX:(g + 1) * W - CTX], xw[:], AF.Sigmoid, reads=["xw"], writes=["sgT"])
            P.tt("dve", T_["kq"][:], T_["xk"][:], kkp[:].unsqueeze(2).to_broadcast([128, 4, W]), ALU.mult,
                 reads=["xk", "kkp"], writes=["kq"])
            P.act(sqb[:], T_["kq"][:], AF.Square, reads=["kq"], writes=["sqb"])
            P.mm(self.pb[0][:, 0:4 * W], self.bd[:], fl(sqb), reads=["sqb", "bd"], writes=["pb0"])
            P.act(fl(T_["rs"]), self.pb[0][:, 0:4 * W], AF.Ln, bias=self.eps6[:, 0:1], reads=["pb0", "eps6"], writes=["rs"])
            P.act(fl(T_["rs"]), fl(T_["rs"]), AF.Exp, scale=-0.5, reads=["rs"], writes=["rs"])
            P.tt("dve", T_["kq"][:], T_["kq"][:], T_["rs"][:], ALU.mult, reads=["kq", "rs"], writes=["kq"])
            for pr in range(4):
                P.mm(self.pb[1][:, pr * W:(pr + 1) * W], wupb[:, d, pr * 128:(pr + 1) * 128], twl[:, :],
                     reads=["wupb", "twl"], writes=["pb1"])
            for pr in range(4):
                P.mm(self.pb[2][:, pr * W:(pr + 1) * W], aupb[:, d, pr * 128:(pr + 1) * 128], tal[:, :],
                     reads=["aupb", "tal"], writes=["pb2"])
            for pr in range(4):
                P.act(T_["sig"][:, pr, :], self.pb[1][:, pr * W:(pr + 1) * W], AF.Sigmoid,
                      bias=w0T[:, d, pr:pr + 1], reads=["pb1", "w0T"], writes=["sig"])
                P.act(T_["aa"][:, pr, :], self.pb[2][:, pr * W:(pr + 1) * W], AF.Sigmoid,
                      bias=a0T[:, d, pr:pr + 1], reads=["pb2", "a0T"], writes=["aa"])
            P.op("dve", lambda: nc.vector.tensor_tensor_scan(out=fl(T_["cs"]), data0=self.rst[:, 0:4 * W], data1=fl(T_["sig"]),
                                                               initial=0.0, op0=ALU.mult, op1=ALU.add),
                 reads=["sig", "rst"], writes=["cs"])
            P.tt("pool", T_["ce"][:], T_["cs"][:], T_["sig"][:], ALU.subtract, reads=["cs", "sig"], writes=["ce"])
            tot = ch(T_["cs"])[:, :, 63:64]
            P.tt("pool", ch(T_["rem"]), tot.to_broadcast([128, 4 * GS, 64]), ch(T_["cs"]), ALU.subtract,
                 reads=["cs"], writes=["rem"])
            P.tt("pool", T_["remi"][:], T_["rem"][:], T_["sig"][:], ALU.add, reads=["rem", "sig"], writes=["remi"])
            incl, excl, rest = ("cs", "ce", "rem") if d == 0 else ("remi", "rem", "ce")
            P.act(T_["e_in"][:], T_[incl][:], AF.Exp, scale=-KAPPA, reads=[incl], writes=["e_in"])
            P.act(T_["e_ex"][:], T_[excl][:], AF.Exp, scale=-KAPPA, reads=[excl], writes=["e_ex"])
            P.act(T_["e_ip"][:], T_[incl][:], AF.Exp, scale=KAPPA, reads=[incl], writes=["e_ip"])
            P.act(T_["e_rs"][:], T_[rest][:], AF.Exp, scale=-KAPPA, reads=[rest], writes=["e_rs"])
            P.act(gC[d][:].rearrange("p a c -> p (a c)"), ch(T_["cs"])[:, :, 63], AF.Exp, scale=-KAPPA,
                  reads=["cs"], writes=[f"gC{d}"])
            P.tt("dve", T_["ka"][:], T_["kq"][:], T_["aa"][:], ALU.mult, reads=["kq", "aa"], writes=["ka"])
            P.tt("pool", T_["t1"][:], T_["aa"][:], kap[:].unsqueeze(2).to_broadcast([128, 4, W]), ALU.mult,
                 reads=["aa", "kap"], writes=["t1"])
            P.tt("pool", T_["t1"][:], T_["t1"][:], omka[:].unsqueeze(2).to_broadcast([128, 4, W]), ALU.add,
                 reads=["t1", "omka"], writes=["t1"])
            P.tt("dve", T_["kd"][:], T_["xk"][:], T_["t1"][:], ALU.mult, reads=["xk", "t1"], writes=["kd"])
            if not isctx:
                for hf in range(2):
                    P.tt("dve" if hf == 0 else "pool", prodb[:, :, hf, :], T_["xr"][:], T_["kd"][:], ALU.mult,
                         reads=["xr", "kd"], writes=["prodb"])
                for h in range(8):
                    pr, e = h // 2, h % 2
                    P.mm(self.pb[0][:, 384 + h:384 + h + 1], prodb[:, pr, :, :].rearrange("p s t -> p (s t)"), rkp[:, pr, e:e + 1],
                         reads=["prodb", "rkp"], writes=["pb0"])
                rp = slice(par * 64, (par + 1) * 64)
                P.tt("dve", bonus[rp, tl, :], bonus[rp, tl, :], self.pb[0][rp, 384:392], ALU.add,
                     reads=["pb0", "bonus"], writes=["bonus"])
            P.stt(slot(AR[d], 0), ch(T_["kq"]), -1.0, ch(T_["e_ex"]), ALU.mult, ALU.mult,
                  reads=["kq", "e_ex"], writes=[f"AR{d}"])
            P.tt("dve", slot(AR[d], 1), ch(T_["xr"]), ch(T_["e_in"]), ALU.mult, reads=["xr", "e_in"], writes=[f"AR{d}"])
            P.tt("pool", slot(BKt, 0), ch(T_["ka"]), ch(T_["e_ip"]), ALU.mult, reads=["ka", "e_ip"], writes=["BKt"])
            P.tt("pool", slot(BKt, 1), ch(T_["kd"]), ch(T_["e_ip"]), ALU.mult, reads=["kd", "e_ip"], writes=["BKt"])
            P.tt("dve", slot(BHt, 0), ch(T_["ka"]), ch(T_["e_rs"]), ALU.mult, reads=["ka", "e_rs"], writes=["BHt"])
            P.tt("pool", slot(BHt, 1), ch(T_["kd"]), ch(T_["e_rs"]), ALU.mult, reads=["kd", "e_rs"], writes=["BHt"])
            fl5 = lambda t: t.rearrange("p a c s t -> p (a c s t)")
            for ee in range(2):
                r_ = slice(ee * 64, (ee + 1) * 64)
                P.copy("pool", fl5(ARp[d][r_, ee]), fl5(AR[d][r_]), reads=[f"AR{d}"], writes=[f"ARp{d}"])
                P.copy("act", fl5(BKp[d][r_, ee]), fl5(BKt[r_]), reads=["BKt"], writes=[f"BKp{d}"])
                P.copy("pool", fl5(BHp[d][r_, ee]), fl5(BHt[r_]), reads=["BHt"], writes=[f"BHp{d}"])
            P.copy("act", slot(XV[d], 1), ch(T_["xv"]), reads=["xv"], writes=[f"XV{d}"])
            P.copy("act", slot(XV[d], 0), ch(T_["xv"]), reads=["xv"], writes=[f"XV{d}"])

        def chunk_pre(d, cj, c_glob):
            fl3 = lambda t: t.rearrange("p s t -> p (s t)")
            for h in range(8):
                pr, e = h // 2, h % 2
                bk = 1 + h // 4
                P.mm(self.pb[bk][:, (h % 4) * 128:(h % 4 + 1) * 128],
                     fl3(BKp[d][:, e, pr, cj, :, :]), fl3(AR[d][:, pr, cj, :, :]),
                     reads=[f"BKp{d}", f"AR{d}"], writes=[f"pb{bk}"])
            for half in range(2):
                P.tt("dve", G[d][:, half * 4:(half + 1) * 4, :],
                     self.pb[1 + half][:, :].rearrange("p (h q) -> p h q", h=4),
                     self.rmask[d][:].unsqueeze(1).to_broadcast([128, 4, 128]), ALU.mult,
                     reads=[f"pb{1 + half}", f"rmask{d}"], writes=[f"G{d}"])
            bT5 = self.pbT(5)
            bT6 = self.pbT(6)
            for h in range(8):
                P.tr(bT6[0:64, h * 64:(h + 1) * 64], G[d][0:64, h, 0:64], self.identb[0:64, 0:64],
                     reads=[f"G{d}", "identb"], writes=["pb6"])
            for h in range(8):
                pr, e = h // 2, h % 2
                P.tr(bT5[:, h * 128:(h + 1) * 128], fl3(BHp[d][:, e, pr, cj, :, :]), self.identb[:],
                     reads=[f"BHp{d}", "identb"], writes=["pb5"])
            for pr in range(4):
                P.tr(bT6[:, 512 + pr * 128:512 + (pr + 1) * 128], fl3(XV[d][:, pr, cj, :, :]), self.identb[:],
                     reads=[f"XV{d}", "identb"], writes=["pb6"])
            P.copy("act", Nt[d][:], bT6[0:64, 0:512].rearrange("p (h t) -> p h t", h=8), reads=["pb6"], writes=[f"Nt{d}"])
            P.copy("dve", BHtok[d][:], bT5[:, :].rearrange("p (h t) -> p h t", h=8), reads=["pb5"], writes=[f"BHtok{d}"])
            P.copy("act", VZ[d][64:128, :, :], bT6[64:128, 512:1024].rearrange("p (h t) -> p h t", h=8),
                   reads=["pb6"], writes=[f"VZ{d}"])
            P.copy("dve", UV[d][64:128, :, :], bT6[64:128, 512:1024].rearrange("p (h t) -> p h t", h=8),
                   reads=["pb6"], writes=[f"UV{d}"])
            if d == 0 and c_glob >= c.NCC:
                rp = slice((c_glob % 2) * 64, (c_glob % 2 + 1) * 64)
                P.copy("act", vtok[rp, c_glob // 2 - NTC, :], bT6[rp, 512:1024], reads=["pb6"], writes=["vtok"])

        def invert(ds):
            I8 = self.identb[0:64, 0:64].unsqueeze(1).to_broadcast([64, 8, 64])
            cur = {}
            for d in ds:
                P.tt("pool", X[d][:], G[d][0:64, :, 0:64], I8, ALU.add, reads=[f"G{d}", "identb"], writes=[f"X{d}"])
                P.tt("pool", Xt[d][:], Nt[d][:], I8, ALU.add, reads=[f"Nt{d}", "identb"], writes=[f"Xt{d}"])
                cur[d] = (lambda h, d=d: G[d][0:64, h, 0:64], lambda h, d=d: Nt[d][:, h, :], f"G{d}", f"Nt{d}")
            for lvl in range(5):
                last = lvl == 4
                for d in ds:
                    Pf, Ptf, kP, kPt = cur[d]
                    b0, b1 = (1, 2) if d == 0 else (3, 4)
                    for h in range(8):
                        P.mm(self.pb[b0][0:64, h * 64:(h + 1) * 64], Ptf(h), Pf(h), reads=[kP, kPt], writes=[f"pb{b0}"])
                    if not last:
                        for h in range(8):
                            P.mm(self.pb[b1][0:64, h * 64:(h + 1) * 64], Pf(h), Ptf(h), reads=[kP, kPt], writes=[f"pb{b1}"])
                    nP, nPt = Pa[d][lvl % 2], Pb[d][lvl % 2]
                    kn, knt = f"Pa{d}{lvl % 2}", f"Pb{d}{lvl % 2}"
                    P.copy("act", nP[:], self.pb[b0][0:64, :].rearrange("p (h t) -> p h t", h=8),
                           reads=[f"pb{b0}"], writes=[kn])
                    if not last:
                        P.copy("dve", nPt[:], self.pb[b1][0:64, :].rearrange("p (h t) -> p h t", h=8),
                               reads=[f"pb{b1}"], writes=[knt])
                    cur[d] = (lambda h, nP=nP: nP[:, h, :], lambda h, nPt=nPt: nPt[:, h, :], kn, knt)
                for d in ds:
                    Pf, Ptf, kP, kPt = cur[d]
                    b0, b1 = (1, 2) if d == 0 else (3, 4)
                    for h in range(8):
                        P.mm(self.pb[b0][0:64, h * 64:(h + 1) * 64], Xt[d][:, h, :], Pf(h),
                             reads=[f"Xt{d}", kP], writes=[f"pb{b0}"])
                    if not last:
                        for h in range(8):
                            P.mm(self.pb[b1][0:64, h * 64:(h + 1) * 64], Pf(h), Xt[d][:, h, :],
                                 reads=[f"Xt{d}", kP], writes=[f"pb{b1}"])
                    P.tt("dve", X[d][:], X[d][:], self.pb[b0][0:64, :].rearrange("p (h t) -> p h t", h=8), ALU.add,
                         reads=[f"X{d}", f"pb{b0}"], writes=[f"X{d}"])
                    if not last:
                        P.tt("dve", Xt[d][:], Xt[d][:], self.pb[b1][0:64, :].rearrange("p (h t) -> p h t", h=8), ALU.add,
                             reads=[f"Xt{d}", f"pb{b1}"], writes=[f"Xt{d}"])

        def chunk_seq(d, c_glob, cj):
            lat = c_glob >= c.NCC
            kS, kSb = f"Sf{d}", f"Sbf{d}"
            bS = 7
            for h in range(8):
                pr, e = h // 2, h % 2
                o_ = self.pb[bS][0:64, h * 64:(h + 1) * 64]
                P.mm(o_, ARp[d][:, e, pr, cj, 0, :], Sbf[:, d, pr, :], start=True, stop=False,
                     reads=[f"ARp{d}", kSb], writes=[f"pb{bS}"])
                P.mm(o_, G[d][:, h, 0:64], VZ[d][:, h, :], start=False, stop=True,
                     reads=[f"G{d}", f"VZ{d}"], writes=[f"pb{bS}"])
            P.copy("act", RHSb[d][:], self.pb[bS][0:64, :].rearrange("p (h t) -> p h t", h=8),
                   reads=[f"pb{bS}"], writes=[f"RHSb{d}"])
            for h in range(8):
                P.mm(self.pb[bS][0:64, h * 64:(h + 1) * 64], X[d][:, h, :], RHSb[d][:, h, :],
                     reads=[f"X{d}", f"RHSb{d}"], writes=[f"pb{bS}"])
            P.copy("dve", UV[d][0:64, :, :], self.pb[bS][0:64, :].rearrange("p (h t) -> p h t", h=8),
                   reads=[f"pb{bS}"], writes=[f"UV{d}"])
            if lat:
                par = c_glob % 2
                tl = c_glob // 2 - NTC
                for h in range(8):
                    pr, e = h // 2, h % 2
                    if par == 0:
                        o_ = self.pb[bS][0:64, h * 64:(h + 1) * 64]
                        l1 = ARp[d][:, e, pr, cj, 1, :]
                        l2 = G[d][:, h, 64:128]
                    else:
                        o_ = self.pb[bS][:, h * 64:(h + 1) * 64]
                        l1 = ARp[d][:, e, pr, cj, :, :].rearrange("p s t -> p (s t)")
                        l2 = G[d][:, h, :]
                    P.mm(o_, l1, Sbf[:, d, pr, :], start=True, stop=False, reads=[f"ARp{d}", kSb], writes=[f"pb{bS}"])
                    P.mm(o_, l2, UV[d][:, h, :], start=False, stop=True, reads=[f"G{d}", f"UV{d}"], writes=[f"pb{bS}"])
                rp = slice(par * 64, (par + 1) * 64)
                P.tt("dve", yacc[rp, tl, :], yacc[rp, tl, :], self.pb[bS][rp, :], ALU.add,
                     reads=["yacc", f"pb{bS}"], writes=["yacc"])
            for pr in range(4):
                o_ = self.pb[0][:, 256 + pr * 64:256 + (pr + 1) * 64]
                P.mm(o_, BHtok[d][:, 2 * pr, :], UV[d][:, 2 * pr, :], start=True, stop=False,
                     reads=[f"BHtok{d}", f"UV{d}"], writes=["pb0"])
                P.mm(o_, BHtok[d][:, 2 * pr + 1, :], UV[d][:, 2 * pr + 1, :], start=False, stop=True,
                     reads=[f"BHtok{d}", f"UV{d}"], writes=["pb0"])
            P.tt("dve", Sf[:, d, :, :], Sf[:, d, :, :], gC[d][:, :, cj:cj + 1].to_broadcast([128, 4, 64]), ALU.mult,
                 reads=[kS, f"gC{d}"], writes=[kS])
            P.tt("dve", Sf[:, d, :, :], Sf[:, d, :, :], self.pb[0][:, 256:512].rearrange("p (a v) -> p a v", a=4), ALU.add,
                 reads=[kS, "pb0"], writes=[kS])
            P.copy("act", Sbf[:, d, :, :], Sf[:, d, :, :], reads=[kS], writes=[kSb])

        order_f = list(range(NG))
        order_b = list(range(NGC - 1, -1, -1)) + list(range(NG - 1, NGC - 1, -1))
        if os.environ.get("BASS_TRACE_TOTAL"):
            print("rwkv passes start b", b, "total", getattr(P, "total", 0))
        parts = os.environ.get("RW_PARTS", "pcis")
        nsteps = int(os.environ.get("RW_STEPS", NG))
        for s in range(min(NG, nsteps)):
            gf, gb = order_f[s], order_b[s]
            if "p" in parts:
                prep(0, gf)
                prep(1, gb)
            for j in range(GS):
                cjf, cjb = j, GS - 1 - j
                if "c" in parts:
                    chunk_pre(0, cjf, gf * GS + cjf)
                    chunk_pre(1, cjb, gb * GS + cjb)
                if "i" in parts:
                    invert([0, 1])
                if "s" in parts:
                    chunk_seq(0, gf * GS + cjf, cjf)
                    chunk_seq(1, gb * GS + cjb, cjb)
        self.dump("bonus", bonus[:], [128, NTL, 8], ["bonus"])
        if os.environ.get("BASS_TRACE_TOTAL"):
            print("rwkv passes end b", b, "total", getattr(P, "total", 0))
        P.barrier()
        esp.close()
        sb = sb_outer
        lnxw = sb("lnxw", [128, 512]); lnxb = sb("lnxb", [128, 512]); gupb = sb("gupb", [128, 512], BF16)
        P.dma("sp", lnxw[:], self.lnxw_d, writes=["lnxw"])
        P.dma("sp", lnxb[:], self.lnxb_d, writes=["lnxb"])
        P.dma("sp", wupf[:], self.g_up_d, writes=["wupf"])
        P.copy("dve", gupb[:], wupf[:], reads=["wupf"], writes=["gupb"])
        ymr = sb("ymr", [128, 512], BF16)
        yn = sb("yn", [128, 8, 64]); ysq = sb("ysq", [128, 8, 64])
        st = sb("st", [128, 8]); st2 = sb("st2", [128, 8])
        for tl in range(NTL):
            yv = yacc[:, tl, :].rearrange("p (h c) -> p h c", h=8)
            P.op("dve", lambda: nc.vector.tensor_reduce(out=st[:], in_=yv, axis=AX.X, op=ALU.add),
                 reads=["yacc"], writes=["st"])
            P.ts("dve", st[:], st[:], -1.0 / 64, ALU.mult, reads=["st"], writes=["st"])
            P.tt("dve", yn[:], yv, st[:].unsqueeze(2).to_broadcast([128, 8, 64]), ALU.add,
                 reads=["yacc", "st"], writes=["yn"])
            P.tt("pool", ysq[:], yn[:], yn[:], ALU.mult, reads=["yn"], writes=["ysq"])
            P.op("dve", lambda: nc.vector.tensor_reduce(out=st2[:], in_=ysq[:], axis=AX.X, op=ALU.add),
                 reads=["ysq"], writes=["st2"])
            P.ts("dve", st2[:], st2[:], 1.0 / 64, ALU.mult, 64e-5, ALU.add, reads=["st2"], writes=["st2"])
            P.act(st2[:], st2[:], AF.Sqrt, reads=["st2"], writes=["st2"])
            P.op("dve", lambda: nc.vector.reciprocal(out=st2[:], in_=st2[:]), reads=["st2"], writes=["st2"])
            P.tt("dve", yn[:], yn[:], st2[:].unsqueeze(2).to_broadcast([128, 8, 64]), ALU.mult,
                 reads=["yn", "st2"], writes=["yn"])
            ynf = yn[:].rearrange("p h c -> p (h c)")
            P.tt("pool", ynf, ynf, lnxw[:], ALU.mult, reads=["yn", "lnxw"], writes=["yn"])
            P.tt("pool", ynf, ynf, lnxb[:], ALU.add, reads=["yn", "lnxb"], writes=["yn"])
            P.tt("dve", ysq[:], vtok[:, tl, :].rearrange("p (h c) -> p h c", h=8),
                 bonus[:, tl, :].unsqueeze(2).to_broadcast([128, 8, 64]), ALU.mult,
                 reads=["vtok", "bonus"], writes=["ysq"])
            P.tt("dve", yn[:], yn[:], ysq[:], ALU.add, reads=["yn", "ysq"], writes=["yn"])
            P.mm(self.pb[1][:, :], sgT[:, tl * 128:(tl + 1) * 128], gupb[:], reads=["sgT", "gupb"], writes=["pb1"])
            P.tt("dve", ymr[:], ynf, self.pb[1][:, :], ALU.mult, reads=["yn", "pb1"], writes=["ymr"])
            P.dma("sp", self.ymix_dram[b, tl * 128:(tl + 1) * 128, 0:512], ymr[:], reads=["ymr"], writes=["ymix_dram"])
        P.barrier()


Builder.rwkv = _rwkv


def _gdn_declare(self):
    i = self.inp
    self.w_in_g = i("w_in_g", [1024, 1536])
    self.w_z = i("w_z", [1024, 512])
    self.w_gl = i("w_gl", [1024, 128])
    self.cwT_d = i("cwT", [128, 12, 5])
    self.gpar_d = i("gpar", [128, 4])
    self.gW_d = i("gW", [128, 2, 128])
    self.gmask_d = i("gmask", [128, 2, 2, 64])
    self.onorm_d = i("onorm_rep", [128, 128])
    self.ymix_dram = self.scratch("ymix_dram", [self.cfg.NB, self.cfg.TL, 1024], BF16)


def _gdn(self, b):
    c, P, nc = self.cfg, self.P, self.nc
    NTL, NTC, CTX, TL, T, NCH = c.NTL, c.NTC, c.CTX, c.TL, c.T, c.NCH
    esg = contextlib.ExitStack()
    with esg:
        sbg = lambda n, s, dt=F32: P.sb(n, s, dt, es=esg)
        pTg = sbg("pTg", [128, 12, T], BF16)
        ztok = sbg("ztok", [128, NTL, 512], BF16)
        CM = sbg("CM", [128, T])
        glT = sbg("glT", [128, T])
        blocks = []
        t = 0
        while t < T:
            w = min(512, T - t)
            blocks.append((t, w))
            t += w
        with contextlib.ExitStack() as es1:
            h1T = P.sb("h1T", [128, 8, T], BF16, es=es1)
            self.phase1(b, h1T, es1)
            self.inproj(h1T, self.w_in_g, 12, pTg, "pTg", es1)
            wst = P.sb("wglst", [128, 8, 128], es=es1)
            wgb = P.sb("wglb", [128, 8, 128], BF16, es=es1)
            P.dma("sp", wst[:], self.w_gl.rearrange("(k p) c -> p k c", p=128), writes=["wglst"])
            P.copy("pool", wgb[:], wst[:], reads=["wglst"], writes=["wglb"])
            for bi_, (t0, w) in enumerate(blocks):
                bk = 1 + bi_ % 4
                for k in range(8):
                    P.mm(self.pb[bk][:, 0:w], wgb[:, k, :], h1T[:, k, t0:t0 + w], start=(k == 0), stop=(k == 7),
                         reads=["wglb", "h1T"], writes=[f"pb{bk}"])
                P.copy("act", glT[:, t0:t0 + w], self.pb[bk][:, 0:w], reads=[f"pb{bk}"], writes=["glT"])
            wzst = P.sb("wzst", [128, 8, 512], es=es1)
            wzb = P.sb("wzb", [128, 8, 512], BF16, es=es1)
            P.dma("sp", wzst[:], self.w_z.rearrange("(k p) c -> p k c", p=128), writes=["wzst"])
            P.copy("pool", wzb[:], wzst[:], reads=["wzst"], writes=["wzb"])
            for tl in range(NTL):
                bk = 1 + tl % 4
                tok = slice(CTX + tl * 128, CTX + (tl + 1) * 128)
                for k in range(8):
                    P.mm(self.pb[bk][:, :], h1T[:, k, tok], wzb[:, k, :], start=(k == 0), stop=(k == 7),
                         reads=["wzb", "h1T"], writes=[f"pb{bk}"])
                P.act(ztok[:, tl, :], self.pb[bk][:, :], AF.Silu, reads=[f"pb{bk}"], writes=["ztok"])
            P.barrier()
        cwT = sbg("cwT", [128, 12, 5]); gpar = sbg("gpar", [128, 4]); gW = sbg("gW", [128, 2, 128])
        gmask = sbg("gmask", [128, 2, 2, 64]); onorm = sbg("onorm", [128, 128])
        negA = sbg("negA", [128, 1])
        ones128 = sbg("ones128", [128, 128], BF16)
        for t_sb, t_d, k in ((cwT, self.cwT_d, "cwT"), (gpar, self.gpar_d, "gpar"), (gW, self.gW_d, "gW"),
                             (gmask, self.gmask_d, "gmask"), (onorm, self.onorm_d, "onorm")):
            P.dma("sp", t_sb[:], t_d, writes=[k])
        P.act(negA[:], gpar[:, 1:2], AF.Exp, reads=["gpar"], writes=["negA"])
        P.ts("dve", negA[:], negA[:], -1.0, ALU.mult, reads=["negA"], writes=["negA"])
        P.memset("pool", ones128[:], 1.0, writes=["ones128"])
        SELA = sbg("SELA", [128, 8, 128]); SELB = sbg("SELB", [128, 8, 64]); SELC = sbg("SELC", [128, 8, 64])
        for tsel, base, wd, k in ((SELA, 0, 128, "SELA"), (SELB, 8, 64, "SELB"), (SELC, 32, 64, "SELC")):
            P.memset("pool", tsel[:], 0.0, writes=[k])
            for r in range(8):
                P.aselect(tsel[:, r, :], tsel[:, r, :], [[0, wd]], ALU.not_equal, 1.0, -(base + r), 1,
                          reads=[k], writes=[k])
        with contextlib.ExitStack() as es2:
            tmpc = P.sb("tmpc", [128, T], es=es2)
            sqg = P.sb("sqg", [128, 512], BF16, es=es2)
            rsg = P.sb("rsg", [128, 512], es=es2)
            segs = ((0, CTX), (CTX, T))
            for ti in range(12):
                for (s0, s1) in segs:
                    P.act(tmpc[:, s0:s1], pTg[:, ti, s0:s1], AF.Copy, scale=cwT[:, ti, 2:3],
                          reads=["pTg", "cwT"], writes=["tmpc"])
                    for tap in (0, 1, 3, 4):
                        sh = tap - 2
                        lo = s0 + max(0, -sh)
                        hi = s1 - max(0, sh)
                        P.stt(tmpc[:, lo:hi], pTg[:, ti, lo + sh:hi + sh], cwT[:, ti, tap:tap + 1], tmpc[:, lo:hi],
                              ALU.mult, ALU.add, reads=["pTg", "cwT", "tmpc"], writes=["tmpc"])
                P.act(pTg[:, ti, :], tmpc[:, :], AF.Silu, reads=["tmpc"], writes=["pTg"])
                if ti < 8:
                    qs = 128.0 ** -0.5 if ti < 4 else 1.0
                    for bi_, (t0, w) in enumerate(blocks):
                        bk = 1 + bi_ % 4
                        P.act(sqg[:, 0:w], pTg[:, ti, t0:t0 + w], AF.Square, reads=["pTg"], writes=["sqg"])
                        P.mm(self.pb[bk][:, 0:w], ones128[:], sqg[:, 0:w], reads=["ones128", "sqg"], writes=[f"pb{bk}"])
                        P.act(rsg[:, 0:w], self.pb[bk][:, 0:w], AF.Ln, bias=self.eps6[:, 0:1],
                              reads=[f"pb{bk}", "eps6"], writes=["rsg"])
                        P.act(rsg[:, 0:w], rsg[:, 0:w], AF.Exp, scale=-0.5, reads=["rsg"], writes=["rsg"])
                        P.stt(pTg[:, ti, t0:t0 + w], pTg[:, ti, t0:t0 + w], qs, rsg[:, 0:w], ALU.mult, ALU.mult,
                              reads=["pTg", "rsg"], writes=["pTg"])
            SRC1 = P.sb("SRC1", [128, T], es=es2)
            SRC2 = P.sb("SRC2", [128, T], es=es2)
            gg = P.sb("gg", [8, T], es=es2)
            gcs = P.sb("gcs", [8, T], es=es2)
            sfx = P.sb("sfx", [8, T], es=es2)
            P.memset("pool", SRC1[:], 0.0, writes=["SRC1"])
            P.memset("pool", SRC2[:], 0.0, writes=["SRC2"])
            P.act(gg[:], glT[0:8, :], AF.Exp, bias=gpar[0:8, 0:1], reads=["glT", "gpar"], writes=["gg"])
            P.act(gg[:], gg[:], AF.Ln, bias=self.one1[0:8, 0:1], reads=["gg", "one1"], writes=["gg"])
            P.ts("dve", gg[:], gg[:], negA[0:8, 0:1], ALU.mult, reads=["gg", "negA"], writes=["gg"])
            for (t0, w) in blocks:
                P.op("dve", lambda t0=t0, w=w: nc.vector.tensor_tensor_scan(
                    out=gcs[:, t0:t0 + w], data0=self.rst[0:8, 0:w], data1=gg[:, t0:t0 + w],
                    initial=0.0, op0=ALU.mult, op1=ALU.add), reads=["gg", "rst"], writes=["gcs"])
            c3 = lambda t_: t_.rearrange("p (n t) -> p n t", t=64)
            tot = c3(gcs[:, :])[:, :, 63:64]
            P.tt("dve", c3(sfx[:, :]), tot.to_broadcast([8, NCH, 64]), c3(gcs[:, :]), ALU.subtract,
                 reads=["gcs"], writes=["sfx"])
            P.tt("dve", sfx[:], sfx[:], gg[:], ALU.add, reads=["sfx", "gg"], writes=["sfx"])
            P.ts("dve", SRC1[0:8, :], gcs[:], gpar[0:8, 2:3], ALU.mult, reads=["gcs", "gpar"], writes=["SRC1"])
            P.stt(SRC1[0:8, :], sfx[:], gpar[0:8, 3:4], SRC1[0:8, :], ALU.mult, ALU.add,
                  reads=["sfx", "gpar", "SRC1"], writes=["SRC1"])
            P.tt("dve", c3(SRC2[0:8, :]), tot.to_broadcast([8, NCH, 64]), c3(SRC1[0:8, :]), ALU.subtract,
                 reads=["gcs", "SRC1"], writes=["SRC2"])
            P.act(SRC1[32:40, :], glT[32:40, :], AF.Exp, scale=-1.0, reads=["glT"], writes=["SRC1"])
            P.act(SRC1[32:40, :], SRC1[32:40, :], AF.Ln, bias=self.one1[32:40, 0:1], reads=["SRC1", "one1"], writes=["SRC1"])
            P.ts("dve", SRC1[32:40, :], SRC1[32:40, :], -1.0, ALU.mult, reads=["SRC1"], writes=["SRC1"])
            for bi_, (t0, w) in enumerate(blocks):
                bk = 1 + bi_ % 4
                P.mm(self.pb[bk][:, 0:w], gW[:, 0, :], SRC1[:, t0:t0 + w], start=True, stop=False,
                     reads=["gW", "SRC1"], writes=[f"pb{bk}"])
                P.mm(self.pb[bk][:, 0:w], gW[:, 1, :], SRC2[:, t0:t0 + w], start=False, stop=True,
                     reads=["gW", "SRC2"], writes=[f"pb{bk}"])
                P.copy("act", CM[:, t0:t0 + w], self.pb[bk][:, 0:w], reads=[f"pb{bk}"], writes=["CM"])
            self.dump("CM", CM[:], [128, T], ["CM"])
            P.barrier()
        oacc = sbg("oacc", [128, NTL, 512], BF16)
        Sf = sbg("gSf", [128, 2, 4, 128]); Sb = sbg("gSb", [128, 2, 4, 128], BF16)
        P.memset("pool", oacc[:], 0.0, writes=["oacc"])
        P.memset("pool", Sf[:], 0.0, writes=["gSf0", "gSf1"])
        P.memset("pool", Sb[:], 0.0, writes=["gSb0", "gSb1"])
        E, GG, EC, KQg, CMt, ekb, kgtok, bV, RHS, U, glast = ([] for _ in range(11))
        for d in range(2):
            E.append(sbg(f"E{d}", [64, 4, 2, 64]))
            GG.append(sbg(f"GG{d}", [64, 4, 2, 64], BF16))
            EC.append(sbg(f"EC{d}", [128, 4, 64], BF16))
            KQg.append(sbg(f"KQg{d}", [128, 4, 2, 64], BF16))
            CMt.append(sbg(f"CMt{d}", [64, 128]))
            ekb.append(sbg(f"ekb{d}", [64, 3, 4]))
            kgtok.append(sbg(f"kgtok{d}", [64, 4, 128], BF16))
            bV.append(sbg(f"bV{d}", [64, 4, 128]))
            RHS.append(sbg(f"RHS{d}", [64, 4, 128], BF16))
            U.append(sbg(f"U{d}", [64, 4, 128], BF16))
            glast.append(sbg(f"glast{d}", [128, 4]))
        Nt = sbg("gNt", [64, 8, 64], BF16); X = sbg("gX", [64, 8, 64], BF16); Xt = sbg("gXt", [64, 8, 64], BF16)
        Pa = [sbg(f"gPa{j}", [64, 8, 64], BF16) for j in range(2)]
        Pb = [sbg(f"gPb{j}", [64, 8, 64], BF16) for j in range(2)]
        tmpR = sbg("tmpR", [64, 4, 128])

        def gpre(d, cg):
            cs = slice(cg * 64, (cg + 1) * 64)
            for h in range(4):
                dh = d * 4 + h
                for v in range(2):
                    o_ = self.pb[1][0:64, (h * 2 + v) * 64:(h * 2 + v + 1) * 64]
                    sel = SELC[:, dh, :] if v == 0 else SELA[:, dh, 0:64]
                    P.mm(o_, sel, CM[:, cs], start=True, stop=False, reads=["SELA", "SELC", "CM"], writes=["pb1"])
                    P.mm(o_, CM[:, cs], SELB[:, dh, :], start=False, stop=False, reads=["SELB", "CM"], writes=["pb1"])
                    P.mm(o_, self.identf[:, 0:64], gmask[:, d, v, :], start=False, stop=True,
                         reads=["identf", "gmask"], writes=["pb1"])
            P.act(E[d][:].rearrange("p h v t -> p (h v t)"), self.pb[1][0:64, :], AF.Exp, reads=["pb1"], writes=[f"E{d}"])
            for h in range(4):
                P.mm(self.pb[2][0:64, (h * 2) * 64:(h * 2 + 1) * 64], pTg[:, 4 + h, cs], pTg[:, 4 + h, cs],
                     reads=["pTg"], writes=["pb2"])
                P.mm(self.pb[2][0:64, (h * 2 + 1) * 64:(h * 2 + 2) * 64], pTg[:, 4 + h, cs], pTg[:, h, cs],
                     reads=["pTg"], writes=["pb2"])
            g4 = self.pb[2][0:64, :].rearrange("p (h v t) -> p h v t", h=4, v=2)
            P.stt(GG[d][:, :, 0, :], g4[:, :, 0, :], -1.0, E[d][:, :, 0, :], ALU.mult, ALU.mult,
                  reads=["pb2", f"E{d}"], writes=[f"GG{d}"])
            P.tt("dve", GG[d][:, :, 1, :], g4[:, :, 1, :], E[d][:, :, 1, :], ALU.mult,
                 reads=["pb2", f"E{d}"], writes=[f"GG{d}"])
            bT6 = self.pbT(6)
            for h in range(4):
                P.tr(bT6[0:64, 768 + h * 64:768 + (h + 1) * 64], GG[d][:, h, 0, :], self.identb[0:64, 0:64],
                     reads=[f"GG{d}", "identb"], writes=["pb6"])
            P.copy("act", Nt[:, d * 4:(d + 1) * 4, :], bT6[0:64, 768:1024].rearrange("p (h t) -> p h t", h=4),
                   reads=["pb6"], writes=[f"gNt{d}"])
            for h in range(4):
                P.mm(self.pb[6][:, h * 64:(h + 1) * 64], SELA[:, d * 4 + h, :], CM[:, cs],
                     reads=["SELA", "CM"], writes=["pb6"])
            P.act(EC[d][:].rearrange("p h t -> p (h t)"), self.pb[6][:, 0:256], AF.Exp, reads=["pb6"], writes=[f"EC{d}"])
            col = 63 if d == 0 else 0
            P.act(glast[d][:], self.pb[6][:, 0:256].rearrange("p (h t) -> p h t", h=4)[:, :, col], AF.Exp,
                  reads=["pb6"], writes=[f"glast{d}"])
            P.tt("dve", KQg[d][:, :, 0, :], pTg[:, 4:8, cs], EC[d][:], ALU.mult, reads=["pTg", f"EC{d}"], writes=[f"KQg{d}"])
            P.tt("pool", KQg[d][:, :, 1, :], pTg[:, 0:4, cs], EC[d][:], ALU.mult, reads=["pTg", f"EC{d}"], writes=[f"KQg{d}"])
            bT5 = self.pbT(5)
            for h in range(4):
                P.tr(bT5[0:64, h * 128:(h + 1) * 128], pTg[:, 4 + h, cs], self.identb[:], reads=["pTg", "identb"], writes=["pb5"])
                P.tr(bT5[0:64, 512 + h * 128:512 + (h + 1) * 128], pTg[:, 8 + h, cs], self.identb[:],
                     reads=["pTg", "identb"], writes=["pb5"])
            P.tr(self.pb[6][0:64, 256:384], CM[:, cs], self.identf[:], reads=["CM", "identf"], writes=["pb6"])
            P.copy("act", CMt[d][:], self.pb[6][0:64, 256:384], reads=["pb6"], writes=[f"CMt{d}"])
            P.act(ekb[d][:, 0, :], CMt[d][:, 64 + d * 4:64 + d * 4 + 4], AF.Exp, reads=[f"CMt{d}"], writes=[f"ekb{d}"])
            P.act(ekb[d][:, 1, :], CMt[d][:, 96 + d * 4:96 + d * 4 + 4], AF.Exp, reads=[f"CMt{d}"], writes=[f"ekb{d}"])
            kv = bT5[0:64, :].rearrange("p (s h k) -> p s h k", s=2, h=4)
            P.tt("dve", kgtok[d][:], kv[:, 0, :, :], ekb[d][:, 0, :].unsqueeze(2).to_broadcast([64, 4, 128]), ALU.mult,
                 reads=["pb5", f"ekb{d}"], writes=[f"kgtok{d}"])
            P.tt("dve", bV[d][:], kv[:, 1, :, :], ekb[d][:, 1, :].unsqueeze(2).to_broadcast([64, 4, 128]), ALU.mult,
                 reads=["pb5", f"ekb{d}"], writes=[f"bV{d}"])

        def ginvert():
            I8 = self.identb[0:64, 0:64].unsqueeze(1).to_broadcast([64, 8, 64])
            kN = ["GG0", "GG1"]
            kNt = ["gNt0", "gNt1"]
            Nf = lambda m: GG[m // 4][:, m % 4, 0, :]
            for d in range(2):
                P.tt("pool", X[:, d * 4:(d + 1) * 4, :], GG[d][:, :, 0, :], I8[:, 0:4, :], ALU.add,
                     reads=[f"GG{d}", "identb"], writes=["gX"])
            P.tt("pool", Xt[:], Nt[:], I8, ALU.add, reads=kNt + ["identb"], writes=["gXt"])
            cur = (Nf, lambda m: Nt[:, m, :], kN, kNt)
            b0, b1 = 3, 4
            for lvl in range(5):
                last = lvl == 4
                Pf, Ptf, kP, kPt = cur
                for m in range(8):
                    P.mm(self.pb[b0][0:64, m * 64:(m + 1) * 64], Ptf(m), Pf(m), reads=kP + kPt, writes=[f"pb{b0}"])
                if not last:
                    for m in range(8):
                        P.mm(self.pb[b1][0:64, m * 64:(m + 1) * 64], Pf(m), Ptf(m), reads=kP + kPt, writes=[f"pb{b1}"])
                nP, nPt = Pa[lvl % 2], Pb[lvl % 2]
                kn, knt = [f"gPa{lvl % 2}"], [f"gPb{lvl % 2}"]
                P.copy("act", nP[:], self.pb[b0][0:64, :].rearrange("p (h t) -> p h t", h=8), reads=[f"pb{b0}"], writes=kn)
                if not last:
                    P.copy("dve", nPt[:], self.pb[b1][0:64, :].rearrange("p (h t) -> p h t", h=8), reads=[f"pb{b1}"], writes=knt)
                cur = (lambda m, nP=nP: nP[:, m, :], lambda m, nPt=nPt: nPt[:, m, :], kn, knt)
                Pf, Ptf, kP, kPt = cur
                for m in range(8):
                    P.mm(self.pb[b0][0:64, m * 64:(m + 1) * 64], Xt[:, m, :], Pf(m), reads=["gXt"] + kP, writes=[f"pb{b0}"])
                if not last:
                    for m in range(8):
                        P.mm(self.pb[b1][0:64, m * 64:(m + 1) * 64], Pf(m), Xt[:, m, :], reads=["gXt"] + kP, writes=[f"pb{b1}"])
                P.tt("dve", X[:], X[:], self.pb[b0][0:64, :].rearrange("p (h t) -> p h t", h=8), ALU.add,
                     reads=["gX", f"pb{b0}"], writes=["gX"])
                if not last:
                    P.tt("dve", Xt[:], Xt[:], self.pb[b1][0:64, :].rearrange("p (h t) -> p h t", h=8), ALU.add,
                         reads=["gXt", f"pb{b1}"], writes=["gXt"])

        def gseq(d, cg):
            lat = cg >= c.NCC
            kS, kSb = f"gSf{d}", f"gSb{d}"
            v4 = lambda bank: bank.rearrange("p (h k) -> p h k", h=4)
            for h in range(4):
                P.mm(self.pb[7][0:64, h * 128:(h + 1) * 128], KQg[d][:, h, 0, :], Sb[:, d, h, :],
                     reads=[f"KQg{d}", kSb], writes=["pb7"])
            P.tt("dve", tmpR[:], v4(self.pb[7][0:64, :]), ekb[d][:, 1, :].unsqueeze(2).to_broadcast([64, 4, 128]), ALU.mult,
                 reads=["pb7", f"ekb{d}"], writes=["tmpR"])
            P.tt("pool", RHS[d][:], bV[d][:], tmpR[:], ALU.subtract, reads=[f"bV{d}", "tmpR"], writes=[f"RHS{d}"])
            for h in range(4):
                P.mm(self.pb[7][0:64, h * 128:(h + 1) * 128], X[:, d * 4 + h, :], RHS[d][:, h, :],
                     reads=["gX", f"RHS{d}"], writes=["pb7"])
            P.copy("act", U[d][:], v4(self.pb[7][0:64, :]), reads=["pb7"], writes=[f"U{d}"])
            if lat:
                par = cg % 2
                tl = cg // 2 - NTC
                for h in range(4):
                    if par == 0:
                        o_ = self.pb[7][0:64, h * 128:(h + 1) * 128]
                        l1 = KQg[d][:, h, 1, :]
                        l2 = GG[d][:, h, 1, :]
                    else:
                        o_ = self.pb[7][:, h * 128:(h + 1) * 128]
                        l1 = KQg[d][:, h, :, :].rearrange("p s t -> p (s t)")
                        l2 = GG[d][:, h, :, :].rearrange("p s t -> p (s t)")
                    P.mm(o_, l1, Sb[:, d, h, :], start=True, stop=False, reads=[f"KQg{d}", kSb], writes=["pb7"])
                    P.mm(o_, l2, U[d][:, h, :], start=False, stop=True, reads=[f"GG{d}", f"U{d}"], writes=["pb7"])
                rp = slice(par * 64, (par + 1) * 64)
                P.tt("dve", oacc[rp, tl, :], oacc[rp, tl, :], self.pb[7][rp, :], ALU.add,
                     reads=["oacc", "pb7"], writes=["oacc"])
            for h in range(4):
                P.mm(self.pb[0][:, h * 128:(h + 1) * 128], kgtok[d][:, h, :], U[d][:, h, :],
                     reads=[f"kgtok{d}", f"U{d}"], writes=["pb0"])
            P.tt("dve", Sf[:, d, :, :], Sf[:, d, :, :], glast[d][:].unsqueeze(2).to_broadcast([128, 4, 128]), ALU.mult,
                 reads=[kS, f"glast{d}"], writes=[kS])
            P.tt("dve", Sf[:, d, :, :], Sf[:, d, :, :], v4(self.pb[0][:, :]), ALU.add, reads=[kS, "pb0"], writes=[kS])
            P.copy("act", Sb[:, d, :, :], Sf[:, d, :, :], reads=[kS], writes=[kSb])

        order_f = list(range(NCH))
        order_b = list(range(c.NCC - 1, -1, -1)) + list(range(NCH - 1, c.NCC - 1, -1))
        nsteps = int(os.environ.get("GD_STEPS", NCH))
        for s_ in range(min(NCH, nsteps)):
            cf, cb = order_f[s_], order_b[s_]
            gpre(0, cf)
            gpre(1, cb)
            ginvert()
            gseq(0, cf)
            gseq(1, cb)
        osq = sbg("osq", [128, 4, 128]); of = sbg("of", [128, 4, 128]); st = sbg("gst", [128, 4])
        yg = sbg("yg", [128, 512], BF16)
        for tl in range(NTL):
            ov = oacc[:, tl, :].rearrange("p (h k) -> p h k", h=4)
            P.tt("pool", osq[:], ov, ov, ALU.mult, reads=["oacc"], writes=["osq"])
            P.op("dve", lambda: nc.vector.tensor_reduce(out=st[:], in_=osq[:], axis=AX.X, op=ALU.add),
                 reads=["osq"], writes=["gst"])
            P.ts("dve", st[:], st[:], 1.0 / 128, ALU.mult, 1e-6, ALU.add, reads=["gst"], writes=["gst"])
            P.act(st[:], st[:], AF.Sqrt, reads=["gst"], writes=["gst"])
            P.op("dve", lambda: nc.vector.reciprocal(out=st[:], in_=st[:]), reads=["gst"], writes=["gst"])
            P.tt("dve", of[:], ov, st[:].unsqueeze(2).to_broadcast([128, 4, 128]), ALU.mult,
                 reads=["oacc", "gst"], writes=["of"])
            P.tt("pool", of[:], of[:], onorm[:].unsqueeze(1).to_broadcast([128, 4, 128]), ALU.mult,
                 reads=["of", "onorm"], writes=["of"])
            P.tt("dve", yg[:], of[:].rearrange("p h k -> p (h k)"), ztok[:, tl, :], ALU.mult,
                 reads=["of", "ztok"], writes=["yg"])
            P.dma("sp", self.ymix_dram[b, tl * 128:(tl + 1) * 128, 512:1024], yg[:], reads=["yg"], writes=["ymix_dram"])
        if "ymix_g" in c.debug:
            of2 = sbg("of2", [128, NTL, 512])
            P.copy("dve", of2[:], oacc[:], reads=["oacc"], writes=["of2"])
            self.dump("oacc", of2[:], [128, NTL, 512], ["of2"])
        P.barrier()


Builder.gdn = _gdn
Builder.gdn_declare = _gdn_declare


def _post_declare(self):
    c = self.cfg
    i = self.inp
    self.w_out_d = i("w_out", [1024, 1024])
    self.router_w_d = i("router_w", [1024, 256])
    self.router_b_d = i("router_b_rep", [128, 256])
    self.ecap_d = i("ecap", [128, 256])
    self.sw1_d = i("sh_w1", [1024, 256])
    self.sw3_d = i("sh_w3", [1024, 256])
    self.sw2_d = i("sh_w2", [256, 1024])
    self.ew1_d = i("exp_w1", [256, 1024, 256])
    self.ew3_d = i("exp_w3", [256, 1024, 256])
    self.ew2_d = i("exp_w2", [256, 256, 1024])
    self.fg_d = i("final_g_rep", [128, 1024])
    NTOK = c.NB * c.TL
    self.NSLOT = 256 * c.CAP
    self.HALF = self.NSLOT // 2
    self.xg_dram = [self.scratch(f"xg_dram{q}", [self.HALF, 1024], BF16) for q in range(2)]
    self.yexp_dram = [self.scratch(f"yexp_dram{q}", [self.HALF, 1024], BF16) for q in range(2)]
    self.hres_dram = self.scratch("hres_dram", [NTOK, 1024], F32)
    self.out_d = self.outp("out", [NTOK, 1024], F32)


def _post_setup(self):
    c, P, nc = self.cfg, self.P, self.nc
    NT = c.NB * c.NTL
    self.cnt_rep = P.sb("cnt_rep", [128, 256])
    self.dest8 = P.sb("dest8", [128, 2, NT, 8], I32)
    self.wsel8 = P.sb("wsel8", [128, NT, 8])
    self.UT = P.sb("UT", [128, 128], BF16)
    self.onesb = P.sb("onesb", [128, 128], BF16)
    self.breg = nc.gpsimd.to_reg(self.HALF - 1)
    P.memset("pool", self.cnt_rep[:], 0.0, writes=["cnt_rep"])
    P.memset("pool", self.onesb[:], 1.0, writes=["onesb"])
    P.memset("pool", self.UT[:], 1.0, writes=["UT"])
    P.aselect(self.UT[:], self.UT[:], [[1, 128]], ALU.is_gt, 0.0, 0, -1, reads=["UT"], writes=["UT"])


def _post(self, b):
    c, P, nc = self.cfg, self.P, self.nc
    NTL, TL, CAP = c.NTL, c.TL, c.CAP
    es = contextlib.ExitStack()
    with es:
        sb = lambda n, s, dt=F32: P.sb(n, s, dt, es=es)
        woutb = sb("woutb", [128, 8, 1024], BF16)
        rwf = sb("rwf", [128, 8, 256])
        sw1b = sb("sw1b", [128, 8, 256], BF16); sw3b = sb("sw3b", [128, 8, 256], BF16)
        sw2b = sb("sw2b", [128, 2, 1024], BF16)
        rb = sb("rb", [128, 256]); ecap = sb("ecap", [128, 256])
        reps = sb("reps", [128, 4, 1024])
        with contextlib.ExitStack() as esw:
            stg = P.sb("stg", [128, 8, 1024], es=esw)
            P.dma("sp", stg[:], self.w_out_d.rearrange("(k p) c -> p k c", p=128), writes=["stg"])
            P.copy("pool", woutb[:], stg[:], reads=["stg"], writes=["woutb"])
            P.dma("sp", stg[:, :, 0:256], self.sw1_d.rearrange("(k p) c -> p k c", p=128), writes=["stg"])
            P.copy("act", sw1b[:], stg[:, :, 0:256], reads=["stg"], writes=["sw1b"])
            P.dma("sp", stg[:, :, 256:512], self.sw3_d.rearrange("(k p) c -> p k c", p=128), writes=["stg"])
            P.copy("act", sw3b[:], stg[:, :, 256:512], reads=["stg"], writes=["sw3b"])
            P.dma("sp", stg[:, 0:2, :], self.sw2_d.rearrange("(k p) c -> p k c", p=128), writes=["stg"])
            P.copy("pool", sw2b[:], stg[:, 0:2, :], reads=["stg"], writes=["sw2b"])
            P.barrier()
        P.dma("sp", rwf[:], self.router_w_d.rearrange("(k p) c -> p k c", p=128), writes=["rwf"])
        P.dma("sp", rb[:], self.router_b_d, writes=["rb"])
        P.dma("sp", ecap[:], self.ecap_d, writes=["ecap"])
        for j in range(4):
            P.dma("sp", reps[:, j, :], self.modscr[b:b + 1, j, :].to_broadcast([128, 1024]), writes=["reps"])
        ymb = sb("ymb", [128, 1024], BF16); yT = sb("yT", [128, 8, 128], BF16)
        xt = sb("pxt", [128, 1024]); h = sb("ph", [128, 1024]); h2 = sb("ph2", [128, 1024])
        h2b = sb("ph2b", [128, 1024], BF16); h2Tf = sb("h2Tf", [128, 8, 128]); h2Tb = sb("h2Tb", [128, 8, 128], BF16)
        junk = sb("pjunk", [128, 1024], BF16); ss = sb("pss", [128, 1])
        sc = sb("sc", [128, 256]); sel = sb("sel", [128, 256]); m8 = sb("m8", [128, 8, 8]); gs = sb("gs", [128, 8])
        g8 = sb("g8", [128, 8]); gm = sb("gm", [128, 8]); e8 = sb("e8", [128, 8]); em = sb("em", [128, 256])
        emb = sb("emb", [128, 256], BF16); wn = sb("wn", [128, 256]); ws = sb("pws", [128, 1])
        pos = sb("pos", [128, 256]); code = sb("code", [128, 256]); c8 = sb("c8", [128, 8]); t8 = sb("t8", [128, 8])
        cj = sb("cj", [128, 256])
        gT = sb("gT", [128, 2, 128], BF16); gtmp = sb("gtmp", [128, 2, 128])
        BIGV = float(self.NSLOT + 8)
        for tl in range(NTL):
            gt_ = b * NTL + tl
            rows = slice(b * TL + tl * 128, b * TL + (tl + 1) * 128)
            P.dma("sp", ymb[:], self.ymix_dram[b, tl * 128:(tl + 1) * 128, :], reads=["ymix_dram"], writes=["ymb"])
            P.dma("sp", xt[:], self.x[b, tl * 128:(tl + 1) * 128, :], writes=["pxt"])
            bT = self.pbT(5)
            for k in range(8):
                P.tr(bT[:, k * 128:(k + 1) * 128], ymb[:, k * 128:(k + 1) * 128], self.identb[:],
                     reads=["ymb", "identb"], writes=["pb5"])
            P.copy("act", yT[:].rearrange("p k t -> p (k t)"), bT[:, :], reads=["pb5"], writes=["yT"])
            for hf in range(2):
                for k in range(8):
                    P.mm(self.pb[1 + hf][:, :], yT[:, k, :], woutb[:, k, hf * 512:(hf + 1) * 512], start=(k == 0), stop=(k == 7),
                         reads=["yT", "woutb"], writes=[f"pb{1 + hf}"])
                P.tt("dve", h[:, hf * 512:(hf + 1) * 512], self.pb[1 + hf][:, :], reps[:, 0, hf * 512:(hf + 1) * 512], ALU.mult,
                     reads=[f"pb{1 + hf}", "reps"], writes=["ph"])
            P.tt("pool", h[:], h[:], xt[:], ALU.add, reads=["ph", "pxt"], writes=["ph"])
            P.act(junk[:], h[:], AF.Square, accum_out=ss[:], reads=["ph"], writes=["pjunk", "pss"])
            P.ts("dve", ss[:], ss[:], 1.0 / 1024, ALU.mult, 1e-6, ALU.add, reads=["pss"], writes=["pss"])
            P.act(ss[:], ss[:], AF.Sqrt, reads=["pss"], writes=["pss"])
            P.op("dve", lambda: nc.vector.reciprocal(out=ss[:], in_=ss[:]), reads=["pss"], writes=["pss"])
            P.stt(h2[:], h[:], ss[:, 0:1], reps[:, 1, :], ALU.mult, ALU.mult, reads=["ph", "pss", "reps"], writes=["ph2"])
            P.tt("pool", h2[:], h2[:], reps[:, 2, :], ALU.add, reads=["ph2", "reps"], writes=["ph2"])
            P.copy("act", h2b[:], h2[:], reads=["ph2"], writes=["ph2b"])
            for half in range(2):
                for k4 in range(4):
                    k = half * 4 + k4
                    P.tr(self.pb[3 + half][:, k4 * 128:(k4 + 1) * 128], h2[:, k * 128:(k + 1) * 128], self.identf[:],
                         reads=["ph2", "identf"], writes=[f"pb{3 + half}"])
                P.copy("act", h2Tf[:, half * 4:(half + 1) * 4, :].rearrange("p k t -> p (k t)"), self.pb[3 + half][:, :],
                       reads=[f"pb{3 + half}"], writes=["h2Tf"])
            P.copy("pool", h2Tb[:], h2Tf[:], reads=["h2Tf"], writes=["h2Tb"])
            for k in range(8):
                P.mm(self.pb[6][:, 0:256], h2Tf[:, k, :], rwf[:, k, :], start=(k == 0), stop=(k == 7),
                     reads=["h2Tf", "rwf"], writes=["pb6"])
            P.act(sc[:], self.pb[6][:, 0:256], AF.Sigmoid, reads=["pb6"], writes=["sc"])
            P.tt("dve", sel[:], sc[:], rb[:], ALU.add, reads=["sc", "rb"], writes=["sel"])
            for g in range(8):
                P.op("dve", lambda g=g: nc.vector.max(out=m8[:, g, :], in_=sel[:, g * 32:(g + 1) * 32]), reads=["sel"], writes=["m8"])
            P.tt("dve", gs[:], m8[:, :, 0], m8[:, :, 1], ALU.add, reads=["m8"], writes=["gs"])
            P.op("dve", lambda: nc.vector.max(out=g8[:], in_=gs[:]), reads=["gs"], writes=["g8"])
            P.ts("dve", gm[:], gs[:], g8[:, 3:4], ALU.is_ge, reads=["gs", "g8"], writes=["gm"])
            P.ts("dve", gm[:], gm[:], 1.0e4, ALU.mult, -1.0e4, ALU.add, reads=["gm"], writes=["gm"])
            P.tt("dve", sel[:].rearrange("p (g e) -> p g e", g=8), sel[:].rearrange("p (g e) -> p g e", g=8),
                 gm[:].unsqueeze(2).to_broadcast([128, 8, 32]), ALU.add, reads=["sel", "gm"], writes=["sel"])
            P.op("dve", lambda: nc.vector.max(out=e8[:], in_=sel[:]), reads=["sel"], writes=["e8"])
            P.ts("dve", em[:], sel[:], e8[:, 7:8], ALU.is_ge, reads=["sel", "e8"], writes=["em"])
            P.stt(wn[:], sc[:], 1.0, em[:], ALU.mult, ALU.mult, accum_out=ws[:], reads=["sc", "em"], writes=["wn", "pws"])
            P.op("dve", lambda: nc.vector.reciprocal(out=ws[:], in_=ws[:]), reads=["pws"], writes=["pws"])
            P.ts("dve", wn[:], wn[:], ws[:, 0:1], ALU.mult, 2.5, ALU.mult, reads=["wn", "pws"], writes=["wn"])
            P.copy("act", emb[:], em[:], reads=["em"], writes=["emb"])
            P.mm(self.pb[6][:, 256:512], self.UT[:], emb[:], reads=["UT", "emb"], writes=["pb6"])
            P.tt("dve", pos[:], self.pb[6][:, 256:512], self.cnt_rep[:], ALU.add, reads=["pb6", "cnt_rep"], writes=["pos"])
            P.mm(self.pb[6][:, 256:512], self.onesb[:], emb[:], reads=["onesb", "emb"], writes=["pb6"])
            P.tt("dve", self.cnt_rep[:], self.cnt_rep[:], self.pb[6][:, 256:512], ALU.add, reads=["pb6", "cnt_rep"], writes=["cnt_rep"])
            P.ts("dve", cj[:], pos[:], float(CAP), ALU.is_lt, reads=["pos"], writes=["cj"])
            P.tt("dve", cj[:], cj[:], em[:], ALU.mult, reads=["cj", "em"], writes=["cj"])
            P.tt("dve", code[:], pos[:], ecap[:], ALU.add, reads=["pos", "ecap"], writes=["code"])
            P.tt("dve", code[:], code[:], cj[:], ALU.mult, reads=["code", "cj"], writes=["code"])
            P.op("dve", lambda: nc.vector.max(out=c8[:], in_=code[:]), reads=["code"], writes=["c8"])
            for k in range(8):
                P.stt(cj[:], code[:], c8[:, k:k + 1], wn[:], ALU.is_equal, ALU.mult, accum_out=self.wsel8[:, gt_, k:k + 1],
                      reads=["code", "c8", "wn"], writes=["cj", "wsel8"])
            P.ts("dve", t8[:], c8[:], 0.0, ALU.is_equal, BIGV, ALU.mult, reads=["c8"], writes=["t8"])
            P.stt(t8[:], c8[:], -1.0, t8[:], ALU.add, ALU.add, reads=["c8", "t8"], writes=["t8"])
            P.ts("dve", c8[:], t8[:], float(self.HALF), ALU.is_ge, BIGV, ALU.mult, reads=["t8"], writes=["c8"])
            P.tt("dve", c8[:], c8[:], t8[:], ALU.add, reads=["c8", "t8"], writes=["c8"])
            P.copy("dve", self.dest8[:, 0, gt_, :], c8[:], reads=["c8"], writes=["dest8"])
            P.ts("dve", c8[:], t8[:], float(self.HALF), ALU.is_lt, 2.0 * BIGV, ALU.mult, reads=["t8"], writes=["c8"])
            P.stt(c8[:], t8[:], -float(self.HALF), c8[:], ALU.add, ALU.add, reads=["c8", "t8"], writes=["c8"])
            P.copy("dve", self.dest8[:, 1, gt_, :], c8[:], reads=["c8"], writes=["dest8"])
            for k in range(8):
                for q in range(2):
                    P.dma("pool", self.xg_dram[q][:, :], h2b[:], reads=["ph2b", "dest8"], writes=[f"xg_dram{q}"],
                          indirect=dict(out_offset=bass.IndirectOffsetOnAxis(ap=self.dest8[:, q, gt_, k:k + 1], axis=0), in_offset=None,
                                        bounds_check=self.breg, oob_is_err=False))
            for j in range(2):
                for wi, wsb in enumerate((sw1b, sw3b)):
                    for k in range(8):
                        P.mm(self.pb[1 + wi][:, j * 128:(j + 1) * 128], wsb[:, k, j * 128:(j + 1) * 128], h2Tb[:, k, :],
                             start=(k == 0), stop=(k == 7), reads=["sw1b", "sw3b", "h2Tb"], writes=[f"pb{1 + wi}"])
            P.act(gtmp[:].rearrange("p j t -> p (j t)"), self.pb[1][:, 0:256], AF.Silu, reads=["pb1"], writes=["gtmp"])
            P.tt("dve", gT[:].rearrange("p j t -> p (j t)"), gtmp[:].rearrange("p j t -> p (j t)"), self.pb[2][:, 0:256], ALU.mult,
                 reads=["gtmp", "pb2"], writes=["gT"])
            for hf in range(2):
                for j in range(2):
                    P.mm(self.pb[3 + hf][:, :], gT[:, j, :], sw2b[:, j, hf * 512:(hf + 1) * 512], start=(j == 0), stop=(j == 1),
                         reads=["gT", "sw2b"], writes=[f"pb{3 + hf}"])
                P.tt("dve", h2[:, hf * 512:(hf + 1) * 512], self.pb[3 + hf][:, :], reps[:, 3, hf * 512:(hf + 1) * 512], ALU.mult,
                     reads=[f"pb{3 + hf}", "reps", "ph2"], writes=["ph2"])
            P.tt("pool", h2[:], h2[:], h[:], ALU.add, reads=["ph2", "ph"], writes=["ph2"])
            P.dma("sp", self.hres_dram[rows, :], h2[:], reads=["ph2"], writes=["hres_dram"])
        P.barrier()


def _moe(self):
    c, P, nc = self.cfg, self.P, self.nc
    CAP = c.CAP
    NST = CAP // 128
    groups = []
    s0 = 0
    while s0 < NST:
        n = min(3, NST - s0)
        groups.append((s0, n))
        s0 += n
    es = contextlib.ExitStack()
    with es:
        sb = lambda n, s, dt=F32: P.sb(n, s, dt, es=es)
        st13 = [sb(f"st13_{j}", [128, 8, 512]) for j in range(2)]
        st2 = [sb(f"st2_{j}", [128, 2, 1024]) for j in range(2)]
        w13b = [sb(f"w13b{j}", [128, 8, 512], BF16) for j in range(2)]
        w2b = [sb(f"w2b{j}", [128, 2, 1024], BF16) for j in range(2)]
        xg = [sb(f"xg{j}", [128, 1024], BF16) for j in range(2)]
        xT = sb("mxT", [128, 8, 384], BF16)
        gtmp = sb("mgtmp", [128, 2, 384]); gT = sb("mgT", [128, 2, 384], BF16)
        ys = [sb(f"ys{j}", [128, 1024], BF16) for j in range(2)]
        nx = 0
        ny = 0
        for e in range(256):
            j = e % 2
            P.dma("sp", st13[j][:, :, 0:256], self.ew1_d[e].rearrange("(k p) c -> p k c", p=128), writes=[f"st13_{j}"])
            P.dma("sp", st13[j][:, :, 256:512], self.ew3_d[e].rearrange("(k p) c -> p k c", p=128), writes=[f"st13_{j}"])
            P.dma("sp", st2[j][:], self.ew2_d[e].rearrange("(k p) c -> p k c", p=128), writes=[f"st2_{j}"])
            P.copy("pool", w13b[j][:, 0:4, :], st13[j][:, 0:4, :], reads=[f"st13_{j}"], writes=[f"w13b{j}"])
            P.copy("act", w13b[j][:, 4:8, :], st13[j][:, 4:8, :], reads=[f"st13_{j}"], writes=[f"w13b{j}"])
            P.copy("pool", w2b[j][:], st2[j][:], reads=[f"st2_{j}"], writes=[f"w2b{j}"])
            for (s0, n) in groups:
                N = n * 128
                for s in range(n):
                    xb = xg[nx % 2]
                    kx = f"xg{nx % 2}"
                    nx += 1
                    q_ = e // 128
                    r0 = (e % 128) * CAP + (s0 + s) * 128
                    P.dma("sp", xb[:], self.xg_dram[q_][r0:r0 + 128, :], reads=[f"xg_dram{q_}"], writes=[kx])
                    bT = self.pbT(5)
                    for k in range(8):
                        P.tr(bT[:, k * 128:(k + 1) * 128], xb[:, k * 128:(k + 1) * 128], self.identb[:],
                             reads=[kx, "identb"], writes=["pb5"])
                    P.copy("act" if s % 2 == 0 else "dve", xT[:, :, s * 128:(s + 1) * 128],
                           bT[:, :].rearrange("p (k t) -> p k t", k=8), reads=["pb5"], writes=["mxT"])
                for jj in range(2):
                    for wi in range(2):
                        bk = 1 + wi * 2 + jj
                        for k in range(8):
                            P.mm(self.pb[bk][:, 0:N], w13b[j][:, k, wi * 256 + jj * 128:wi * 256 + (jj + 1) * 128], xT[:, k, 0:N],
                                 start=(k == 0), stop=(k == 7), reads=[f"w13b{j}", "mxT"], writes=[f"pb{bk}"])
                    P.act(gtmp[:, jj, 0:N], self.pb[1 + jj][:, 0:N], AF.Silu, reads=[f"pb{1 + jj}"], writes=["mgtmp"])
                    P.tt("dve", gT[:, jj, 0:N], gtmp[:, jj, 0:N], self.pb[3 + jj][:, 0:N], ALU.mult,
                         reads=["mgtmp", f"pb{3 + jj}"], writes=["mgT"])
                for s in range(n):
                    yb = ys[ny % 2]
                    ky = f"ys{ny % 2}"
                    ny += 1
                    for hf in range(2):
                        bk = 6 + hf
                        for jj in range(2):
                            P.mm(self.pb[bk][:, :], gT[:, jj, s * 128:(s + 1) * 128], w2b[j][:, jj, hf * 512:(hf + 1) * 512],
                                 start=(jj == 0), stop=(jj == 1), reads=["mgT", f"w2b{j}"], writes=[f"pb{bk}"])
                        P.copy("act" if hf == 0 else "dve", yb[:, hf * 512:(hf + 1) * 512], self.pb[bk][:, :],
                               reads=[f"pb{bk}"], writes=[ky])
                    q_ = e // 128
                    r0 = (e % 128) * CAP + (s0 + s) * 128
                    P.dma("sp", self.yexp_dram[q_][r0:r0 + 128, :], yb[:], reads=[ky], writes=[f"yexp_dram{q_}"])
        P.barrier()


def _final(self):
    c, P, nc = self.cfg, self.P, self.nc
    NTL, TL = c.NTL, c.TL
    es = contextlib.ExitStack()
    with es:
        sb = lambda n, s, dt=F32: P.sb(n, s, dt, es=es)
        fg = sb("fg", [128, 1024]); gt2 = sb("fgt2", [128, 1024])
        P.dma("sp", fg[:], self.fg_d, writes=["fg"])
        hr = [sb(f"hr{j}", [128, 1024]) for j in range(2)]
        yk = [sb(f"yk{j}", [128, 1024], BF16) for j in range(4)]
        acc = sb("facc", [128, 1024]); junk = sb("fjunk", [128, 1024], BF16); ss = sb("fss", [128, 1])
        ng = 0
        for b in range(c.NB):
            P.dma("sp", gt2[:], self.modscr[b:b + 1, 3, :].to_broadcast([128, 1024]), writes=["fgt2"])
            for tl in range(NTL):
                gt_ = b * NTL + tl
                rows = slice(b * TL + tl * 128, b * TL + (tl + 1) * 128)
                hb = hr[gt_ % 2]
                kh = f"hr{gt_ % 2}"
                P.dma("sp", hb[:], self.hres_dram[rows, :], reads=["hres_dram"], writes=[kh])
                for k in range(8):
                    yb = yk[ng % 4]
                    ky = f"yk{ng % 4}"
                    ng += 1
                    P.memset("pool", yb[:], 0.0, writes=[ky])
                    for q in range(2):
                        P.dma("pool", yb[:], self.yexp_dram[q][:, :], reads=[f"yexp_dram{q}", "dest8"], writes=[ky],
                              indirect=dict(out_offset=None, in_offset=bass.IndirectOffsetOnAxis(ap=self.dest8[:, q, gt_, k:k + 1], axis=0),
                                            bounds_check=self.breg, oob_is_err=False))
                    if k == 0:
                        P.ts("dve", acc[:], yb[:], self.wsel8[:, gt_, 0:1], ALU.mult, reads=[ky, "wsel8"], writes=["facc"])
                    else:
                        P.stt(acc[:], yb[:], self.wsel8[:, gt_, k:k + 1], acc[:], ALU.mult, ALU.add,
                              reads=[ky, "wsel8", "facc"], writes=["facc"])
                P.tt("dve", acc[:], acc[:], gt2[:], ALU.mult, reads=["facc", "fgt2"], writes=["facc"])
                P.tt("pool", acc[:], acc[:], hb[:], ALU.add, reads=["facc", kh], writes=["facc"])
                P.act(junk[:], acc[:], AF.Square, accum_out=ss[:], reads=["facc"], writes=["fjunk", "fss"])
                P.ts("dve", ss[:], ss[:], 1.0 / 1024, ALU.mult, 1e-6, ALU.add, reads=["fss"], writes=["fss"])
                P.act(ss[:], ss[:], AF.Sqrt, reads=["fss"], writes=["fss"])
                P.op("dve", lambda: nc.vector.reciprocal(out=ss[:], in_=ss[:]), reads=["fss"], writes=["fss"])
                P.stt(hb[:], acc[:], ss[:, 0:1], fg[:], ALU.mult, ALU.mult, reads=["facc", "fss", "fg"], writes=[kh])
                P.dma("sp", self.out_d[rows, :], hb[:], reads=[kh], writes=["out"])
        P.barrier()


Builder.post_declare = _post_declare
Builder.post_setup = _post_setup
Builder.post = _post
Builder.moe = _moe
Builder.final = _final


_CACHE = {}


def kernel(**inputs):
    cfg = Cfg(NB=1, ROWS=32, CTX=256, CAP=768)
    if "b" not in _CACHE:
        B = Builder(cfg)
        B.build()
        _CACHE["b"] = B
    B = _CACHE["b"]
    shared = prep_shared(inputs, cfg)
    shared = {k: v for k, v in shared.items() if k in B.din}
    nb = int(np.asarray(inputs["x"]).shape[0])
    outs = []
    for l in range(nb // 8):
        in_maps = []
        for core in range(8):
            m = prep_core(inputs, cfg, l * 8 + core, shared)
            in_maps.append({k: v for k, v in m.items() if k in B.din})
        res = run_bass_kernel_spmd(B.nc, in_maps, core_ids=list(range(8)))
        outs.extend(np.asarray(r["out"]).reshape(1, cfg.TL, 1024) for r in res.results)
    return np.concatenate(outs, axis=0).astype(np.float32)
```
